# Optimizing a Trainium2 kernel written in Bass

```python
import math
import jax, jax.numpy as jnp
from jax import lax
import numpy as np

D_MODEL = 1024
BATCH = 8
SEQ = 8192
DEPTH = 4

MEM_LEN = 256
D_MIX = D_MODEL
D_RWKV = D_MIX // 2
D_CONV = D_MIX - D_RWKV
RWKV_HEAD = 64
RWKV_HEADS = D_RWKV // RWKV_HEAD
CONV_WIDTH = 31
DECAY_LORA = 64
AAA_LORA = 64
MV_LORA = 32
GATE_LORA = 128
XA_HEADS = 4
XA_HEAD_DIM = D_MODEL // XA_HEADS
D_FF = -(-8 * D_MODEL // (3 * 256)) * 256
RMS_EPS = 1e-6
LN_EPS = 1e-5
GN_EPS = 64e-5
DECAY_SCALE = math.exp(-0.5)

N_RWKV_COLS = 3 * D_RWKV + DECAY_LORA + AAA_LORA + GATE_LORA
N_IN_FIRST = 2 * D_CONV + N_RWKV_COLS
N_IN_REST = N_IN_FIRST + MV_LORA

kernel_name = "hymba_rwkv7_conformer_memxattn_trunk"


def rms_norm(x, g):
    xf = x.astype(jnp.float32)
    y = xf * lax.rsqrt(jnp.mean(xf * xf, axis=-1, keepdims=True) + RMS_EPS)
    return (y * g.astype(jnp.float32)).astype(x.dtype)


def layer_norm(x, g, b):
    xf = x.astype(jnp.float32)
    mean = jnp.mean(xf, axis=-1, keepdims=True)
    var = jnp.mean(jnp.square(xf - mean), axis=-1, keepdims=True)
    y = (xf - mean) * lax.rsqrt(var + LN_EPS)
    return (y * g.astype(jnp.float32) + b.astype(jnp.float32)).astype(x.dtype)


def token_shift_mix(p, mu):
    prev = jnp.pad(p, ((0, 0), (1, 0), (0, 0)))[:, :-1]
    return p + mu * (prev - p)


def conformer_conv(u, conv_w, conv_b, ln_g, ln_b):
    h = u[..., :D_CONV] * jax.nn.sigmoid(u[..., D_CONV:])
    h = lax.conv_general_dilated(
        h, conv_w[:, None, :], window_strides=(1,), padding=[(CONV_WIDTH - 1, 0)],
        dimension_numbers=("NWC", "WIO", "NWC"), feature_group_count=D_CONV) + conv_b
    h = layer_norm(h, ln_g, ln_b)
    return jax.nn.silu(h)


def rwkv7_recurrence(r, w, k, v, kk, a):
    B = r.shape[0]
    tm = lambda z: jnp.moveaxis(z.astype(jnp.float32), 1, 0)

    def step(S, inp):
        r_t, w_t, k_t, v_t, kk_t, a_t = inp
        s_kk = jnp.einsum("bhvk,bhk->bhv", S, kk_t)
        S = (S * w_t[:, :, None, :]
             - s_kk[..., None] * (kk_t * a_t)[:, :, None, :]
             + v_t[..., None] * k_t[:, :, None, :])
        return S, jnp.einsum("bhvk,bhk->bhv", S, r_t)

    S0 = jnp.zeros((B, RWKV_HEADS, RWKV_HEAD, RWKV_HEAD), jnp.float32)
    _, y = lax.scan(step, S0, (tm(r), tm(w), tm(k), tm(v), tm(kk), tm(a)))
    return jnp.moveaxis(y, 0, 1)


def rwkv7_mix(p, mu, w0, w_up, a0, a_up, g_up, k_k, k_a, r_k, lnx_g, lnx_b, v_first, v0, v_up):
    B, T = p.shape[:2]
    p = token_shift_mix(p, mu)
    o = 0
    r = p[..., o:o + D_RWKV]; o += D_RWKV
    k = p[..., o:o + D_RWKV]; o += D_RWKV
    v = p[..., o:o + D_RWKV]; o += D_RWKV
    w_lo = p[..., o:o + DECAY_LORA]; o += DECAY_LORA
    a_lo = p[..., o:o + AAA_LORA]; o += AAA_LORA
    g_lo = p[..., o:o + GATE_LORA]; o += GATE_LORA

    log_w = -DECAY_SCALE * jax.nn.sigmoid((w0 + jnp.tanh(w_lo) @ w_up).astype(jnp.float32))
    w = jnp.exp(log_w)
    a = jax.nn.sigmoid(a0 + a_lo @ a_up)
    g = jax.nn.sigmoid(g_lo) @ g_up
    if v_first is None:
        v_first = v
    else:
        vres_lo = p[..., o:o + MV_LORA]
        v = v + (v_first - v) * jax.nn.sigmoid(v0 + vres_lo @ v_up)

    hd = lambda z: z.reshape(B, T, RWKV_HEADS, RWKV_HEAD)
    kk = hd(k * k_k).astype(jnp.float32)
    kk = kk * lax.rsqrt(jnp.maximum(jnp.sum(kk * kk, axis=-1, keepdims=True), 1e-24))
    k = k * (1.0 + (a - 1.0) * k_a)
    rh, kh, vh = hd(r), hd(k), hd(v)

    y = rwkv7_recurrence(rh, hd(w), kh, vh, kk, hd(a))
    mean = jnp.mean(y, axis=-1, keepdims=True)
    var = jnp.mean(jnp.square(y - mean), axis=-1, keepdims=True)
    y = (y - mean) * lax.rsqrt(var + GN_EPS)
    gn_g = lnx_g.reshape(RWKV_HEADS, RWKV_HEAD).astype(jnp.float32)
    gn_b = lnx_b.reshape(RWKV_HEADS, RWKV_HEAD).astype(jnp.float32)
    y = y * gn_g + gn_b
    bonus = jnp.sum((rh * kh * r_k).astype(jnp.float32), axis=-1, keepdims=True) * vh.astype(jnp.float32)
    y = (y + bonus).astype(p.dtype).reshape(B, T, D_RWKV) * g
    return y, v_first


def memory_cross_attention(h, mem_n, wq, wkv, wo):
    B, T, _ = h.shape
    M = mem_n.shape[1]
    q = (h @ wq).reshape(B, T, XA_HEADS, XA_HEAD_DIM)
    kv = mem_n @ wkv
    km = kv[..., :D_MODEL].reshape(B, M, XA_HEADS, XA_HEAD_DIM)
    vm = kv[..., D_MODEL:].reshape(B, M, XA_HEADS, XA_HEAD_DIM)
    s = jnp.einsum("bthd,bmhd->bhtm", q, km).astype(jnp.float32) * (XA_HEAD_DIM ** -0.5)
    pr = jax.nn.softmax(s, axis=-1).astype(vm.dtype)
    o = jnp.einsum("bhtm,bmhd->bthd", pr, vm).reshape(B, T, D_MODEL)
    return o @ wo


def swiglu(h, w_gu, w_down):
    gu = h @ w_gu
    return (jax.nn.silu(gu[..., :D_FF]) * gu[..., D_FF:]) @ w_down


def setup_inputs(seed: int = 0) -> dict:
    key = jax.random.key(seed)
    ks = iter(jax.random.split(key, 40))
    nrm = lambda shape, s: jax.random.normal(next(ks), shape, jnp.float32) * s
    uni = lambda shape: jax.random.uniform(next(ks), shape, jnp.float32)
    L, Lr = DEPTH, DEPTH - 1
    return {
        "x": nrm((BATCH, SEQ, D_MODEL), 1.0),
        "mem": nrm((BATCH, MEM_LEN, D_MODEL), 1.0),
        "mem_norm_g": 1.0 + nrm((D_MODEL,), 0.05),
        "norm_gains": 1.0 + nrm((L, 6, D_MODEL), 0.05),
        "w_in_first": nrm((D_MODEL, N_IN_FIRST), D_MODEL ** -0.5),
        "w_in_rest": nrm((Lr, D_MODEL, N_IN_REST), D_MODEL ** -0.5),
        "mu_first": uni((N_RWKV_COLS,)),
        "mu_rest": uni((Lr, N_RWKV_COLS + MV_LORA)),
        "conv_w": nrm((L, CONV_WIDTH, D_CONV), CONV_WIDTH ** -0.5),
        "conv_b": nrm((L, D_CONV), 0.02),
        "conv_ln_g": 1.0 + nrm((L, D_CONV), 0.05),
        "conv_ln_b": nrm((L, D_CONV), 0.02),
        "w0": nrm((L, D_RWKV), 1.0),
        "w_up": nrm((L, DECAY_LORA, D_RWKV), DECAY_LORA ** -0.5),
        "a0": nrm((L, D_RWKV), 0.1),
        "a_up": nrm((L, AAA_LORA, D_RWKV), AAA_LORA ** -0.5),
        "g_up": nrm((L, GATE_LORA, D_RWKV), GATE_LORA ** -0.5),
        "v0": nrm((Lr, D_RWKV), 0.1),
        "v_up": nrm((Lr, MV_LORA, D_RWKV), MV_LORA ** -0.5),
        "k_k": 0.85 + nrm((L, D_RWKV), 0.05),
        "k_a": 1.0 + nrm((L, D_RWKV), 0.05),
        "r_k": nrm((L, RWKV_HEADS, RWKV_HEAD), 0.1),
        "lnx_g": 1.0 + nrm((L, D_RWKV), 0.05),
        "lnx_b": nrm((L, D_RWKV), 0.02),
        "w_out": nrm((L, D_MIX, D_MODEL), D_MIX ** -0.5),
        "wq": nrm((L, D_MODEL, D_MODEL), D_MODEL ** -0.5),
        "wkv": nrm((L, D_MODEL, 2 * D_MODEL), D_MODEL ** -0.5),
        "wo": nrm((L, D_MODEL, D_MODEL), D_MODEL ** -0.5),
        "w_gu": nrm((L, D_MODEL, 2 * D_FF), D_MODEL ** -0.5),
        "w_down": nrm((L, D_FF, D_MODEL), D_FF ** -0.5),
    }


def reference(x, mem, mem_norm_g, norm_gains, w_in_first, w_in_rest, mu_first, mu_rest,
              conv_w, conv_b, conv_ln_g, conv_ln_b, w0, w_up, a0, a_up, g_up, v0, v_up,
              k_k, k_a, r_k, lnx_g, lnx_b, w_out, wq, wkv, wo, w_gu, w_down):
    mem_n = rms_norm(mem, mem_norm_g)
    v_first = None
    for i in range(DEPTH):
        g = norm_gains[i]
        h = rms_norm(x, g[0])
        if i == 0:
            p = h @ w_in_first
            mu, vr0, vr_up = mu_first, None, None
        else:
            p = h @ w_in_rest[i - 1]
            mu, vr0, vr_up = mu_rest[i - 1], v0[i - 1], v_up[i - 1]
        y_conv = conformer_conv(p[..., :2 * D_CONV], conv_w[i], conv_b[i], conv_ln_g[i], conv_ln_b[i])
        y_rwkv, v_first = rwkv7_mix(p[..., 2 * D_CONV:], mu, w0[i], w_up[i], a0[i], a_up[i], g_up[i],
                                    k_k[i], k_a[i], r_k[i], lnx_g[i], lnx_b[i], v_first, vr0, vr_up)
        y = jnp.concatenate([y_conv, y_rwkv], axis=-1) @ w_out[i]
        x = x + rms_norm(y, g[1])
        y = memory_cross_attention(rms_norm(x, g[2]), mem_n, wq[i], wkv[i], wo[i])
        x = x + rms_norm(y, g[3])
        y = swiglu(rms_norm(x, g[4]), w_gu[i], w_down[i])
        x = x + rms_norm(y, g[5])
    return x
```

```python
import numpy as np
import concourse.bass as bass
import concourse.mybir as mybir
from concourse.bass_utils import run_bass_kernel_spmd
from contextlib import ExitStack

F32 = mybir.dt.float32
BF16 = mybir.dt.bfloat16
AF = mybir.ActivationFunctionType
ALU = mybir.AluOpType

D = 1024
KC = 8
SEQ = 8192
DEPTH = 4
MEM = 256
DFF = 2816
NFF = DFF // 128
CW = 31
DECAY_SCALE = float(np.exp(-0.5))
NSLOT = 6
SAME_ENGINE_WINDOW = 1
STAGE = 3
DEBUG = False


class Buf:
    __slots__ = ("w", "r")

    def __init__(self):
        self.w = None
        self.r = {}


class T:
    __slots__ = ("ap", "bufs")

    def __init__(self, ap, bufs=None):
        self.ap = ap
        if bufs is None:
            bufs = [Buf()]
        elif isinstance(bufs, Buf):
            bufs = [bufs]
        self.bufs = bufs

    def v(self, ap):
        return T(ap, self.bufs)

    def __getitem__(self, k):
        return T(self.ap[k], self.bufs)


class Sched:
    ENG = ("pe", "act", "dve", "pool", "sp")

    def __init__(self, nc, es):
        self.nc = nc
        self.es = es
        self.h = {"pe": nc.tensor, "act": nc.scalar, "dve": nc.vector, "pool": nc.gpsimd, "sp": nc.sync}
        self.ops = {k: [] for k in self.ENG}
        self.sig = {k: [] for k in self.ENG}
        self.dma_cnt = {}
        self.dma_keys = []

    def new_dma_sem(self, key):
        self.dma_cnt[key] = 0
        self.dma_keys.append(key)

    def _collect(self, reads, writes):
        deps = {}

        def add(tok):
            if tok is None:
                return
            k, v = tok
            if deps.get(k, 0) < v:
                deps[k] = v
        for b in reads:
            add(b.w)
        for b in writes:
            add(b.w)
            for k, v in b.r.items():
                add((k, v))
        return deps

    def op(self, eng, fn, reads=(), writes=(), dma=None):
        deps = self._collect(reads, writes)
        idx = len(self.ops[eng]) + 1
        fdeps = {}
        for k, v in deps.items():
            if k == eng:
                if eng == "pe" or eng == "sp" or dma is not None:
                    continue
                if v < idx - SAME_ENGINE_WINDOW:
                    continue
            fdeps[k] = v
            if k in self.sig:
                self.sig[k][v - 1] = True
        self.ops[eng].append((fn, fdeps, dma))
        self.sig[eng].append(False)
        if dma is not None:
            self.dma_cnt[dma] += 1
            tok = (dma, self.dma_cnt[dma])
        else:
            tok = (eng, idx)
        for b in reads:
            k, v = tok
            if b.r.get(k, 0) < v:
                b.r[k] = v
        for b in writes:
            b.w = tok
            b.r = {}
        return tok

    def emit(self):
        nc = self.nc
        sem = {k: self.es.enter_context(nc.semaphore("s_" + k)) for k in self.ENG}
        for k in self.dma_keys:
            sem[k] = self.es.enter_context(nc.semaphore("d_" + k))
        val = {}
        for e in self.ENG:
            c = 0
            arr = []
            for s in self.sig[e]:
                if s:
                    c += 1
                arr.append(c)
            val[e] = arr
        n_ins = 0
        for e in self.ENG:
            h = self.h[e]
            waited = {}
            for i, (fn, deps, dma) in enumerate(self.ops[e]):
                for k, v in deps.items():
                    if k in val:
                        sv = val[k][v - 1]
                    else:
                        sv = 16 * v
                    if waited.get(k, 0) >= sv:
                        continue
                    h.wait_ge(sem[k], sv)
                    waited[k] = sv
                ins = fn(h)
                n_ins += 1
                if dma is not None:
                    ins.then_inc(sem[dma], 16)
                elif self.sig[e][i]:
                    ins.then_inc(sem[e], 1)
        self.sem = sem
        return n_ins


def _unit(W, cols, k0, k1):
    sub = W[k0 * 128:k1 * 128][:, cols]
    sub = sub.reshape(k1 - k0, 128, sub.shape[1]).transpose(1, 0, 2)
    return np.ascontiguousarray(sub).reshape(128, -1)


def _vec(v):
    return np.ascontiguousarray(v.reshape(-1, 128).T)


def w_in_unit_cols(l):
    units = []
    units.append(("lo0", np.arange(2560, 2688)))
    units.append(("lo1", np.arange(2688, 2816)))
    if l > 0:
        units.append(("lo2", np.arange(2816, 2848)))
    for j in range(4):
        units.append((f"r{j}", np.arange(1024 + j * 128, 1024 + (j + 1) * 128)))
        units.append((f"k{j}", np.arange(1536 + j * 128, 1536 + (j + 1) * 128)))
        units.append((f"v{j}", np.arange(2048 + j * 128, 2048 + (j + 1) * 128)))
    for i in range(4):
        units.append((f"ca{i}", np.arange(i * 128, (i + 1) * 128)))
        units.append((f"cg{i}", np.arange(512 + i * 128, 512 + (i + 1) * 128)))
    return units


def layer_units(l):
    out = []
    for name, cols in w_in_unit_cols(l):
        out.append(("in_" + name, 8 * len(cols)))
    for n in range(8):
        out.append((f"out{n}", 1024))
    for n in range(8):
        out.append((f"q{n}", 1024))
    for n in range(8):
        out.append((f"o{n}", 1024))
    for i in range(NFF):
        out.append((f"g{i}", 1024))
        out.append((f"u{i}", 1024))
    for n in range(8):
        out.append((f"d{n}a", 1024))
        out.append((f"d{n}b", 1024))
        out.append((f"d{n}c", 6 * 128))
    return out


def prep_layer_weights(l, inp):
    w_in = inp["w_in_first"] if l == 0 else inp["w_in_rest"][l - 1]
    parts = []
    for name, cols in w_in_unit_cols(l):
        parts.append(_unit(w_in, cols, 0, 8))
    for W in (inp["w_out"][l], inp["wq"][l], inp["wo"][l]):
        for n in range(8):
            parts.append(_unit(W, np.arange(n * 128, (n + 1) * 128), 0, 8))
    wgu = inp["w_gu"][l]
    for i in range(NFF):
        parts.append(_unit(wgu, np.arange(i * 128, (i + 1) * 128), 0, 8))
        parts.append(_unit(wgu, np.arange(DFF + i * 128, DFF + (i + 1) * 128), 0, 8))
    wd = inp["w_down"][l]
    for n in range(8):
        cols = np.arange(n * 128, (n + 1) * 128)
        parts.append(_unit(wd, cols, 0, 8))
        parts.append(_unit(wd, cols, 8, 16))
        parts.append(_unit(wd, cols, 16, 22))
    return np.ascontiguousarray(np.concatenate(parts, axis=1))


PC = {}
_o = 0
for _name, _n in (("g", 48), ("mu", 15), ("cw", 124), ("cb", 4), ("lng", 4), ("lnb", 4), ("w0", 4), ("a0", 4),
                  ("v0", 4), ("kk", 4), ("ka", 4), ("rk", 4), ("xg", 4), ("xb", 4)):
    PC[_name] = _o
    _o += _n
NPAR = _o


def prep_params(l, inp):
    P = np.zeros((128, NPAR), np.float32)
    g = inp["norm_gains"][l]
    for i in range(6):
        P[:, PC["g"] + i * 8: PC["g"] + (i + 1) * 8] = _vec(g[i])
    mu = inp["mu_first"] if l == 0 else inp["mu_rest"][l - 1]
    mu_p = np.zeros(15 * 128, np.float32)
    mu_p[:mu.shape[0]] = mu
    P[:, PC["mu"]:PC["mu"] + 15] = _vec(mu_p)
    cw = inp["conv_w"][l]
    P[:, PC["cw"]:PC["cw"] + 124] = np.ascontiguousarray(
        cw.reshape(CW, 4, 128).transpose(2, 1, 0)).reshape(128, 124)
    for name, key in (("cb", "conv_b"), ("lng", "conv_ln_g"), ("lnb", "conv_ln_b"), ("w0", "w0"), ("a0", "a0"),
                      ("kk", "k_k"), ("ka", "k_a"), ("xg", "lnx_g"), ("xb", "lnx_b")):
        P[:, PC[name]:PC[name] + 4] = _vec(inp[key][l])
    P[:, PC["rk"]:PC["rk"] + 4] = _vec(inp["r_k"][l].reshape(-1))
    if l > 0:
        P[:, PC["v0"]:PC["v0"] + 4] = _vec(inp["v0"][l - 1])
    return P


def prep_lora(l, inp):
    A = np.zeros((128, 3, 512), np.float32)
    A[0:64, 0] = inp["w_up"][l]
    A[64:128, 0] = inp["a_up"][l]
    A[:, 1] = inp["g_up"][l]
    if l > 0:
        A[0:32, 2] = inp["v_up"][l - 1]
    return A


CONST_W = 1412


def make_consts():
    C = np.zeros((128, CONST_W), np.float32)
    p = np.arange(128)
    h = p // 64
    j = p % 64
    same = (h[:, None] == h[None, :])
    C[:, 0:128] = np.eye(128)
    mus = (same & (j[:, None] < j[None, :])).astype(np.float32)
    C[:, 128:256] = mus
    C[:, 256:384] = -mus
    mls = (same & (j[None, :] < j[:, None])).astype(np.float32)
    C[:, 384:512] = -mls
    t = np.arange(64)
    mui = (j[:, None] <= t[None, :]).astype(np.float32)
    C[:, 512:576] = mui
    C[:, 576:640] = -mui
    C[:, 640:768] = same.astype(np.float32) / 64.0
    C[:, 768:896] = same.astype(np.float32)
    C[:, 896:1024] = 1.0 / 1024.0
    C[:, 1024:1152] = 1.0 / 512.0
    C[:, 1152:1280] = 1.0
    C[:, 1280:1408] = -np.eye(128)
    C[:, 1408] = 1e-6
    C[:, 1409] = 1e-5
    C[:, 1410] = 64e-5
    C[:, 1411] = 1e-24
    return C


def build(NT=SEQ // 256, NL=DEPTH, TT=256):
    NCH = TT // 64
    nc = bass.Bass("TRN2", target_bir_lowering=False)
    es = ExitStack()
    S = Sched(nc, es)

    xT = nc.dram_tensor("xT", [D, NT * TT], F32, kind="ExternalInput").ap()
    memT = nc.dram_tensor("memT", [D, MEM], F32, kind="ExternalInput").ap()
    memg = nc.dram_tensor("memg", [128, 8], F32, kind="ExternalInput").ap()
    consts_d = nc.dram_tensor("consts", [128, CONST_W], F32, kind="ExternalInput").ap()
    par_d = nc.dram_tensor("par", [NL, 128, NPAR], F32, kind="ExternalInput").ap()
    lora_d = nc.dram_tensor("lora", [NL, 128, 3 * 512], F32, kind="ExternalInput").ap()
    wkv_d = nc.dram_tensor("wkvu", [NL, 128, 16 * 1024], F32, kind="ExternalInput").ap()
    lunits = [layer_units(l) for l in range(NL)]
    wl_d = [nc.dram_tensor(f"wl{l}", [128, sum(n for _, n in lunits[l])], F32, kind="ExternalInput").ap()
            for l in range(NL)]
    outT = nc.dram_tensor("outT", [D, NT * TT], F32, kind="ExternalOutput").ap()
    if DEBUG:
        dbg_d = nc.dram_tensor("dbg", [128, KC * TT], F32, kind="ExternalOutput").ap()

    def sb(name, shape, dt):
        return es.enter_context(nc.sbuf_tensor(name, shape, dt))

    def ps_t(name, shape, dt):
        return es.enter_context(nc.psum_tensor(name, shape, dt))

    X = sb("X", [128, KC, TT], F32)
    Xc = [T(X[:, c, :]) for c in range(KC)]
    Hh = sb("H", [128, KC, TT], BF16)
    Hc = [T(Hh[:, c, :]) for c in range(KC)]
    MIXb = sb("MIX", [128, KC, TT], BF16)
    MIXc = [T(MIXb[:, c, :]) for c in range(KC)]
    Y = sb("Y", [128, KC, TT], F32)
    Yc = [T(Y[:, c, :]) for c in range(KC)]
    KT = [sb(f"KT{l}", [128, KC, MEM], BF16) for l in range(NL)]
    KTt = [T(KT[l][:]) for l in range(NL)]
    VM = [sb(f"VM{l}", [128, 2, D], BF16) for l in range(NL)]
    VMt = [T(VM[l][:]) for l in range(NL)]
    RING = sb("ring", [128, NSLOT, 1024], BF16)
    ring = [T(RING[:, s, :]) for s in range(NSLOT)]
    VF = sb("VF", [128, 4, TT], F32)
    VFc = [T(VF[:, j, :]) for j in range(4)]
    HS = [[T(sb(f"HS{l}_{j}", [128, 128], F32)[:]) for j in range(4)] for l in range(NL)]
    HSb = [[[T(sb(f"HSb{l}_{j}_{k}", [128, 128], BF16)[:]) for k in range(2)] for j in range(4)] for l in range(NL)]
    hsb_par = [[0] * 4 for _ in range(NL)]
    PAR = [T(sb(f"PAR{l}", [128, NPAR], F32)[:]) for l in range(NL)]
    LORA = [T(sb(f"LORA{l}", [128, 3, 512], BF16)[:]) for l in range(NL)]
    CH = [T(sb(f"CH{l}", [128, 4, CW - 1], F32)[:]) for l in range(NL)]
    SH = [T(sb(f"SH{l}", [128, 15], F32)[:]) for l in range(NL)]
    CF = T(sb("CF", [128, CONST_W], F32)[:])
    CB = T(sb("CB", [128, CONST_W], BF16)[:])

    def cf(a, b):
        return CF[:, a:b]

    def cb(a, b):
        return CB[:, a:b]
    IDb = cb(0, 128)
    MUS, NMUS, NMLS = cf(128, 256), cf(256, 384), cf(384, 512)
    MUI, NMUI = cf(512, 576), cf(576, 640)
    IDf = cf(0, 128)
    BO64b, BO1b = cb(640, 768), cb(768, 896)
    O1024b, O512b, O1b = cb(896, 1024), cb(1024, 1152), cb(1152, 1280)

    RSTD = T(sb("RSTD", [128, TT], F32)[:])
    SQ = [T(sb(f"SQ{i}", [128, TT], BF16)[:]) for i in range(2)]
    sq_i = [0]
    PT = [T(sb(f"PT{i}", [128, TT + 1], F32)[:]) for i in range(3)]
    pt_i = [0]
    TMP = [T(sb(f"TMP{i}", [128, TT], F32)[:]) for i in range(4)]
    tmp_i = [0]

    def tmp():
        t = TMP[tmp_i[0] % len(TMP)]
        tmp_i[0] += 1
        return t
    LOb = T(sb("LOb", [128, TT], BF16)[:])
    Gb = T(sb("Gb", [128, TT], BF16)[:])
    VRb = T(sb("VRb", [32, TT], BF16)[:])
    NRAW = 2
    RAW = [[T(sb(f"RAW{i}_{q}", [128, TT], F32)[:]) for q in range(3)] for i in range(NRAW)]
    P0 = sb("P0", [128, NCH, 96], F32)
    P1 = sb("P1", [128, NCH, 96], F32)
    P0t, P1t = T(P0[:]), T(P1[:])
    At = T(sb("A_t", [128, TT], F32)[:])
    KKt = T(sb("KK_t", [128, TT], F32)[:])
    Kpt = T(sb("Kp_t", [128, TT], F32)[:])
    Bvt = T(sb("Bv_t", [128, TT], F32)[:])
    EX = [T(sb(f"EX{i}", [128, NCH, 64], F32)[:]) for i in range(4)]
    GCt = [T(sb(f"GC{i}", [128, NCH], F32)[:]) for i in range(2)]
    NSET = 2
    EXPN = ("KKe", "KTe", "BTe", "KHe", "BHe", "Ve")
    EXP = [{n: T(sb(f"{n}{i}", [128, NCH, 128], BF16)[:]) for n in EXPN} for i in range(NSET)]
    RT = [T(sb(f"Rt{i}", [128, TT], BF16)[:]) for i in range(NSET)]
    Gt = [T(sb(f"G_t{i}", [128, TT], F32)[:]) for i in range(NSET)]
    BON = [T(sb(f"BON{i}", [128, TT], F32)[:]) for i in range(NSET)]
    YR = [T(sb(f"YR{i}", [128, TT], F32)[:]) for i in range(NSET)]
    MATN = ("LkkT", "M1", "N1", "Sa", "Sb", "Ma", "Mb", "Na", "Nb", "Vbd", "KHbd", "BHbdn", "KKbd", "X1", "TKT", "Usb")
    NMSET = 2
    MATS = [{n: T(sb(f"m_{n}{i}", [128, 128], BF16)[:]) for n in MATN} for i in range(NMSET)]
    for i in range(NMSET):
        MATS[i]["MrkT"] = T(sb(f"m_MrkT{i}", [128, 64], BF16)[:])
        MATS[i]["MrbTn"] = T(sb(f"m_MrbTn{i}", [128, 64], BF16)[:])
    mset_i = [0]
    HG = sb("HG", [128, 4, CW - 1 + TT], F32)
    HGc = [T(HG[:, i, :]) for i in range(4)]
    ACC = sb("ACC", [128, 4, TT], F32)
    ACCc = [T(ACC[:, i, :]) for i in range(4)]
    MEANt = T(sb("MEANt", [128, TT], F32)[:])
    VARt = T(sb("VARt", [128, TT], F32)[:])
    Q = sb("Q", [128, KC, TT], BF16)
    Qc = [T(Q[:, c, :]) for c in range(KC)]
    E = [T(sb(f"E{i}", [128, 2, TT], BF16)[:]) for i in range(2)]
    RD = [T(sb(f"RD{i}", [128, TT], F32)[:]) for i in range(2)]
    Oo = sb("Oo", [128, KC, TT], BF16)
    Oc = [T(Oo[:, c, :]) for c in range(KC)]
    AV = sb("AV", [128, NFF, TT], BF16)
    AVc = [T(AV[:, i, :]) for i in range(NFF)]
    MN = T(Hh[:, :, 0:MEM], [b for t in Hc for b in t.bufs])
    MF = T(Y[:, :, 0:MEM], [b for t in Yc for b in t.bufs])
    MG = T(sb("MG", [128, 8], F32)[:])
    VTt = T(sb("VTt", [128, MEM], BF16)[:])

    PSB = [ps_t(f"psb{i}", [128, 512], F32) for i in range(8)]
    PSBT = [T(PSB[b][:]) for b in range(8)]
    pb_i = [0]

    def pbig():
        t = PSBT[pb_i[0] % 8]
        pb_i[0] += 1
        return t[:, 0:TT]

    def psm():
        t = PSBT[pb_i[0] % 8]
        pb_i[0] += 1
        return t[:, 0:128]

    def bufs_of(*ts):
        out = []
        for t in ts:
            if isinstance(t, T):
                out.extend(t.bufs)
        return out

    def apof(x):
        return x.ap if isinstance(x, T) else x

    def MM(out, lhsT, rhs, start=True, stop=True):
        S.op("pe", lambda e: e.matmul(out.ap, lhsT=lhsT.ap, rhs=rhs.ap, start=start, stop=stop),
             bufs_of(lhsT, rhs), out.bufs)

    def ACT(out, in_, func, bias=None, scale=1.0):
        kw = {}
        if bias is not None:
            kw["bias"] = apof(bias)
        S.op("act", lambda e: e.activation(out=out.ap, in_=in_.ap, func=func, scale=apof(scale), **kw),
             bufs_of(in_, bias, scale), out.bufs)

    def TTo(out, in0, in1, op, eng="dve"):
        S.op(eng, lambda e: e.tensor_tensor(out=out.ap, in0=in0.ap, in1=in1.ap, op=op),
             bufs_of(in0, in1), out.bufs)

    def TS(out, in0, s1, op0, s2=None, op1=None, eng="dve"):
        if op1 is None:
            S.op(eng, lambda e: e.tensor_scalar(out=out.ap, in0=in0.ap, scalar1=apof(s1), scalar2=None, op0=op0),
                 bufs_of(in0, s1), out.bufs)
        else:
            S.op(eng, lambda e: e.tensor_scalar(out=out.ap, in0=in0.ap, scalar1=apof(s1), scalar2=apof(s2),
                                                op0=op0, op1=op1),
                 bufs_of(in0, s1, s2), out.bufs)

    def STT(out, in0, sc, in1, op0, op1, eng="dve"):
        S.op(eng, lambda e: e.scalar_tensor_tensor(out=out.ap, in0=in0.ap, scalar=apof(sc), in1=in1.ap,
                                                   op0=op0, op1=op1),
             bufs_of(in0, sc, in1), out.bufs)

    def CP(out, in_, eng="dve"):
        if eng == "act":
            ACT(out, in_, AF.Copy)
        else:
            S.op(eng, lambda e: e.tensor_copy(out=out.ap, in_=in_.ap), in_.bufs, out.bufs)

    def MEMSET(t, val, eng="dve"):
        S.op(eng, lambda e: e.memset(t.ap, val), (), t.bufs)

    def DMA(eng, out, in_ap, key):
        S.op(eng, lambda e: e.dma_start(out=out.ap, in_=in_ap), (), out.bufs, dma=key)

    def DMAOUT(eng, out_ap, in_, key):
        S.op(eng, lambda e: e.dma_start(out=out_ap, in_=in_.ap), in_.bufs, (), dma=key)

    EPSC = {1e-6: 1408, 1e-5: 1409, 64e-5: 1410, 1e-24: 1411}

    def RSQRT(out, in_, eps):
        c = EPSC[eps]
        ACT(out, in_, AF.Sqrt, bias=CF[:, c:c + 1])
        S.op("dve", lambda e: e.reciprocal(out=out.ap, in_=out.ap), out.bufs, out.bufs)

    evac_i = [0]

    def EVAC(out, in_):
        evac_i[0] += 1
        if evac_i[0] % 2:
            CP(out, in_, "act")
        else:
            CP(out, in_, "dve")

    for s in range(NSLOT):
        S.new_dma_sem(f"w{s}")
    wseq = []
    for l in range(NL):
        for n in range(16):
            wseq.append((wkv_d[l, :, n * 1024:(n + 1) * 1024], 1024, f"kv{l}_{n}"))
    for t in range(NT):
        for l in range(NL):
            off = 0
            for name, ncols in lunits[l]:
                wseq.append((wl_d[l][:, off:off + ncols], ncols, f"L{l}_{name}"))
                off += ncols
    wst = {"issue": 0, "use": 0}

    def wget(expect):
        while wst["issue"] < min(len(wseq), wst["use"] + NSLOT):
            u = wst["issue"]
            ap, ncols, _ = wseq[u]
            slot = u % NSLOT
            DMA("pool", ring[slot][:, 0:ncols], ap, f"w{slot}")
            wst["issue"] += 1
        u = wst["use"]
        assert wseq[u][2] == expect, (wseq[u][2], expect)
        wst["use"] += 1
        return ring[u % NSLOT]

    def setup_dma(eng, out, in_ap, key):
        S.new_dma_sem(key)
        DMA(eng, out, in_ap, key)
    for k in ("xin", "xout"):
        S.new_dma_sem(k)
    setup_dma("sp", CF, consts_d[:, :], "cst")
    setup_dma("pool", CB, consts_d[:, :], "cstb")
    for l in range(NL):
        setup_dma("sp", PAR[l], par_d[l], f"par{l}")
    for l in range(NL):
        setup_dma("pool", LORA[l], lora_d[l].rearrange("p (a b) -> p a b", a=3), f"lora{l}")
    setup_dma("sp", MF, memT.rearrange("(c p) m -> p c m", p=128), "mem")
    setup_dma("sp", MG, memg[:, :], "memg")
    for l in range(NL):
        for j in range(4):
            MEMSET(HS[l][j], 0.0)
            MEMSET(HSb[l][j][0], 0.0)
            MEMSET(HSb[l][j][1], 0.0)
        MEMSET(CH[l], 0.0)
        MEMSET(SH[l], 0.0)
    for i in range(NSET):
        for n in EXPN:
            MEMSET(EXP[i][n], 0.0)
    MEMSET(P0t, 0.0)
    MEMSET(P1t, 0.0)

    def sqtile():
        t = SQ[sq_i[0] % 2]
        sq_i[0] += 1
        return t

    def rms_stats(src_list, out_rstd, ncol, ones_b, eps):
        p = pbig()
        n = len(src_list)
        for i, s in enumerate(src_list):
            q = sqtile()
            ACT(q[:, 0:ncol], s, AF.Square)
            MM(p[:, 0:ncol], ones_b, q[:, 0:ncol], start=(i == 0), stop=(i == n - 1))
        RSQRT(out_rstd, p[:, 0:ncol], eps)

    MFc = [MF[:, c, :] for c in range(KC)]
    MRS = T(sb("MRS", [128, MEM], F32)[:])
    rms_stats(MFc, MRS, MEM, O1024b, 1e-6)
    for c in range(KC):
        STT(MN[:, c, :], MF[:, c, :], MG[:, c:c + 1], MRS, ALU.mult, ALU.mult)
    for l in range(NL):
        for n in range(16):
            w = wget(f"kv{l}_{n}")
            p = pbig()
            for kc in range(KC):
                MM(p[:, 0:MEM], w[:, kc * 128:(kc + 1) * 128], MN[:, kc, :], start=(kc == 0), stop=(kc == KC - 1))
            if n < 8:
                EVAC(KTt[l][:, n, :], p[:, 0:MEM])
            else:
                EVAC(VTt, p[:, 0:MEM])
                for mc in range(2):
                    q = psm()
                    MM(q, VTt[:, mc * 128:(mc + 1) * 128], IDb)
                    EVAC(VMt[l][:, mc, (n - 8) * 128:(n - 7) * 128], q)

    def pre_norm(l, gi):
        rms_stats(Xc, RSTD, TT, O1024b, 1e-6)
        for c in range(KC):
            col = PC["g"] + gi * 8 + c
            STT(Hc[c], Xc[c], PAR[l][:, col:col + 1], RSTD, ALU.mult, ALU.mult)

    def post_norm(l, gi):
        rms_stats(Yc, RSTD, TT, O1024b, 1e-6)
        for c in range(KC):
            col = PC["g"] + gi * 8 + c
            t = tmp()
            STT(t, Yc[c], PAR[l][:, col:col + 1], RSTD, ALU.mult, ALU.mult)
            TTo(Xc[c], Xc[c], t, ALU.add)

    def linear8(l, prefix, src, nout, consume):
        for n in range(nout):
            w = wget(f"L{l}_{prefix}{n}")
            p = pbig()
            for kc in range(KC):
                MM(p, w[:, kc * 128:(kc + 1) * 128], src[kc], start=(kc == 0), stop=(kc == KC - 1))
            consume(n, p)

    def rwkv_pre(l, j, raw, si):
        R, K, V = raw
        P = PAR[l]
        ex = EXP[si]
        pc = lambda name: P[:, PC[name] + j:PC[name] + j + 1]
        lora = LORA[l]
        jc = slice(j * 128, (j + 1) * 128)
        v3 = lambda t: t.v(t.ap.rearrange("p (c t) -> p c t", t=64))
        p = pbig()
        MM(p, lora[0:64, 0, jc], LOb[0:64, :])
        t1 = tmp()
        ACT(t1, p, AF.Sigmoid, bias=pc("w0"))
        LG = P0t[:, :, 32:96]
        TS(LG, v3(t1), -DECAY_SCALE, ALU.mult)
        p = pbig()
        MM(p, lora[64:128, 0, jc], LOb[64:128, :])
        ACT(At, p, AF.Sigmoid, bias=pc("a0"))
        p = pbig()
        MM(p, lora[:, 1, jc], Gb)
        CP(Gt[si], p, "act")
        if l == 0:
            CP(VFc[j], V, "dve")
        else:
            p = pbig()
            MM(p, lora[0:32, 2, jc], VRb[0:32, :])
            t2 = tmp()
            ACT(t2, p, AF.Sigmoid, bias=pc("v0"))
            t3 = tmp()
            TTo(t3, VFc[j], V, ALU.subtract)
            TTo(t3, t3, t2, ALU.mult)
            TTo(V, V, t3, ALU.add)
        TS(KKt, K, pc("kk"), ALU.mult)
        q = sqtile()
        ACT(q, KKt, AF.Square)
        p = pbig()
        MM(p, BO1b, q)
        t4 = tmp()
        RSQRT(t4, p, 1e-24)
        TTo(KKt, KKt, t4, ALU.mult)
        t5 = tmp()
        TS(t5, At, pc("ka"), ALU.mult, pc("ka"), ALU.subtract)
        STT(Kpt, t5, 1.0, K, ALU.add, ALU.mult)
        TTo(Bvt, KKt, At, ALU.mult)
        q = sqtile()
        STT(q, R, pc("rk"), Kpt, ALU.mult, ALU.mult)
        p = pbig()
        MM(p, BO1b, q)
        TTo(BON[si], p, V, ALU.mult)
        src, dst = P0t, P1t
        for s in (1, 2, 4, 8, 16, 32):
            TTo(dst[:, :, 32:96], src[:, :, 32:96], src[:, :, 32 - s:96 - s], ALU.add)
            src, dst = dst, src
        assert src is P0t
        LGP = P0t[:, :, 31:95]
        ACT(EX[0], LG, AF.Exp)
        ACT(EX[1], LGP, AF.Exp)
        ACT(EX[2], LG, AF.Exp, scale=-1.0)
        gc = GCt[si]
        ACT(gc, P0t[:, :, 95], AF.Exp)
        TTo(EX[3], EX[2], gc.v(gc.ap.unsqueeze(2).to_broadcast([128, NCH, 64])), ALU.mult)
        TTo(RT[si], R, EX[0].v(EX[0].ap.rearrange("p c t -> p (c t)")), ALU.mult)
        for h in range(2):
            ph = slice(h * 64, (h + 1) * 64)
            fh = slice(h * 64, (h + 1) * 64)
            TTo(ex["KKe"][ph, :, fh], v3(KKt)[ph], EX[1][ph], ALU.mult)
            TTo(ex["KTe"][ph, :, fh], v3(Kpt)[ph], EX[2][ph], ALU.mult)
            TTo(ex["BTe"][ph, :, fh], v3(Bvt)[ph], EX[2][ph], ALU.mult)
            TTo(ex["KHe"][ph, :, fh], v3(Kpt)[ph], EX[3][ph], ALU.mult)
            TTo(ex["BHe"][ph, :, fh], v3(Bvt)[ph], EX[3][ph], ALU.mult)
            CP(ex["Ve"][ph, :, fh], v3(V)[ph], "dve")

    def rwkv_chunk(l, j, c, si):
        ex = EXP[si]
        m = MATS[mset_i[0] % NMSET]
        mset_i[0] += 1
        KKe, KTe, BTe, KHe, BHe, Ve = (ex[n][:, c, :] for n in EXPN)
        Rc = RT[si][:, c * 64:(c + 1) * 64]
        gcol = GCt[si][:, c:c + 1]
        hs = HS[l][j]
        hb_old = HSb[l][j][hsb_par[l][j]]
        hb_new = HSb[l][j][1 - hsb_par[l][j]]
        hsb_par[l][j] = 1 - hsb_par[l][j]
        p = psm(); MM(p, KTe, KKe); TTo(m["LkkT"], p, MUS, ALU.mult)
        p = psm(); MM(p, BTe, KKe); TTo(m["M1"], p, NMUS, ALU.mult)
        p = psm(); MM(p, KKe, BTe); TTo(m["N1"], p, NMLS, ALU.mult)
        p = psm(); MM(p[:, 0:64], KTe, Rc); TTo(m["MrkT"], p[:, 0:64], MUI, ALU.mult)
        p = psm(); MM(p[:, 0:64], BTe, Rc); TTo(m["MrbTn"], p[:, 0:64], NMUI, ALU.mult)
        p = psm(); MM(p, Ve, IDb); EVAC(m["Vbd"], p)
        p = psm(); MM(p, KHe, IDb); EVAC(m["KHbd"], p)
        p = psm(); MM(p, BHe, cb(1280, 1408)); EVAC(m["BHbdn"], p)
        p = psm(); MM(p, KKe, IDb); EVAC(m["KKbd"], p)
        Mc, Nc, Sc = m["M1"], m["N1"], m["Sa"]
        TTo(Sc, Mc, IDf, ALU.add)
        Mn, Nn, Sn = m["Ma"], m["Na"], m["Sb"]
        for lvl in range(5):
            last = (lvl == 4)
            if not last:
                p = psm(); MM(p, Nc, Mc); EVAC(Mn, p)
            p = psm(); MM(p, Mc, Nc); EVAC(Nn, p)
            p = psm(); MM(p, Nn, Sc); TTo(Sn, p, Sc, ALU.add)
            if lvl == 0:
                Mc, Nc, Sc, Mn, Nn, Sn = Mn, Nn, Sn, m["Mb"], m["Nb"], m["Sa"]
            else:
                Mc, Nc, Sc, Mn, Nn, Sn = Mn, Nn, Sn, Mc, Nc, Sc
        TT_ = Sc
        p = psm(); MM(p, m["LkkT"], m["Vbd"]); EVAC(m["X1"], p)
        p = psm(); MM(p, m["KKbd"], TT_); EVAC(m["TKT"], p)
        p = psm()
        MM(p, TT_, m["X1"], start=True, stop=False)
        MM(p, m["TKT"], hb_old, start=False, stop=True)
        CP(m["Usb"], p, "act")
        py = psm()
        MM(py[:, 0:64], hb_old, Rc, start=True, stop=False)
        MM(py[:, 0:64], m["Vbd"], m["MrkT"], start=False, stop=False)
        MM(py[:, 0:64], m["Usb"], m["MrbTn"], start=False, stop=True)
        p = psm()
        MM(p, m["KHbd"], m["Vbd"], start=True, stop=False)
        MM(p, m["BHbdn"], m["Usb"], start=False, stop=True)
        STT(hs, hs, gcol, p, ALU.mult, ALU.add)
        CP(hb_new, hs, "act")
        CP(YR[si][:, c * 64:(c + 1) * 64], py[:, 0:64], "dve")

    def rwkv_post(l, j, si):
        P = PAR[l]
        pc = lambda name: P[:, PC[name] + j:PC[name] + j + 1]
        yr = YR[si]
        q = sqtile()
        CP(q, yr, "act")
        pm = pbig()
        MM(pm, BO64b, q)
        q2 = sqtile()
        ACT(q2, yr, AF.Square)
        pe2 = pbig()
        MM(pe2, BO64b, q2)
        mean = tmp()
        CP(mean, pm, "act")
        m2 = tmp()
        TTo(m2, mean, mean, ALU.mult)
        var = tmp()
        TTo(var, pe2, m2, ALU.subtract)
        RSQRT(var, var, 64e-5)
        t1 = tmp()
        TTo(t1, yr, mean, ALU.subtract)
        TTo(t1, t1, var, ALU.mult)
        TS(t1, t1, pc("xg"), ALU.mult, pc("xb"), ALU.add)
        TTo(t1, t1, BON[si], ALU.add)
        TTo(MIXc[4 + j], t1, Gt[si], ALU.mult)

    def mixer(l):
        P = PAR[l]
        pre_norm(l, 0)
        for i in range(4):
            CP(HGc[i][:, 0:CW - 1], CH[l][:, i, :], "act")
        names = [n for n, _ in w_in_unit_cols(l)]
        raw_i = 0
        state = {}
        rawcur = None
        for name in names:
            w = wget(f"L{l}_in_{name}")
            ncol = 32 if name == "lo2" else 128
            p = pbig()
            for kc in range(KC):
                MM(p[0:ncol, :], w[:, kc * ncol:(kc + 1) * ncol], Hc[kc], start=(kc == 0), stop=(kc == KC - 1))
            kind = name[0]
            if kind in ("l", "r", "k", "v"):
                if kind == "l":
                    midx = 12 + int(name[2])
                else:
                    midx = {"r": 0, "k": 4, "v": 8}[kind] + int(name[1])
                pt = PT[pt_i[0] % len(PT)]
                pt_i[0] += 1
                rows = slice(0, ncol)
                CP(pt[rows, 0:1], SH[l][rows, midx:midx + 1], "act")
                CP(pt[rows, 1:TT + 1], p[rows, :], "act")
                d = tmp()
                TTo(d[rows], pt[rows, 0:TT], pt[rows, 1:TT + 1], ALU.subtract)
                CP(SH[l][rows, midx:midx + 1], pt[rows, TT:TT + 1], "act")
                mucol = P[rows, PC["mu"] + midx:PC["mu"] + midx + 1]
                if kind == "l":
                    sh = tmp()
                    STT(sh[rows], d[rows], mucol, pt[rows, 1:TT + 1], ALU.mult, ALU.add)
                    if name == "lo0":
                        ACT(LOb[0:64, :], sh[0:64, :], AF.Tanh)
                        CP(LOb[64:128, :], sh[64:128, :], "act")
                    elif name == "lo1":
                        ACT(Gb, sh, AF.Sigmoid)
                    else:
                        CP(VRb[0:32, :], sh[0:32, :], "act")
                else:
                    j = int(name[1])
                    if kind == "r":
                        rawcur = RAW[raw_i % NRAW]
                        raw_i += 1
                    dst = rawcur[{"r": 0, "k": 1, "v": 2}[kind]]
                    STT(dst, d, mucol, pt[:, 1:TT + 1], ALU.mult, ALU.add)
                    if kind == "v":
                        si = j % NSET
                        rwkv_pre(l, j, rawcur, si)
                        for c in range(NCH):
                            rwkv_chunk(l, j, c, si)
                        rwkv_post(l, j, si)
            else:
                i = int(name[2])
                if name[1] == "a":
                    state["pa"] = p
                else:
                    sg = tmp()
                    ACT(sg, p, AF.Sigmoid)
                    TTo(HGc[i][:, CW - 1:CW - 1 + TT], state["pa"], sg, ALU.mult)
        for i in range(4):
            CP(CH[l][:, i, :], HGc[i][:, TT:TT + CW - 1], "act")
        for jj in range(CW):
            for i in range(4):
                wcol = P[:, PC["cw"] + i * CW + jj:PC["cw"] + i * CW + jj + 1]
                if jj == 0:
                    TS(ACCc[i], HGc[i][:, 0:TT], wcol, ALU.mult, P[:, PC["cb"] + i:PC["cb"] + i + 1], ALU.add)
                else:
                    STT(ACCc[i], HGc[i][:, jj:jj + TT], wcol, ACCc[i], ALU.mult, ALU.add)
        pm = pbig()
        pe2 = pbig()
        for i in range(4):
            q = sqtile()
            CP(q, ACCc[i], "act")
            MM(pm, O512b, q, start=(i == 0), stop=(i == 3))
            q2 = sqtile()
            ACT(q2, ACCc[i], AF.Square)
            MM(pe2, O512b, q2, start=(i == 0), stop=(i == 3))
        CP(MEANt, pm, "act")
        m2 = tmp()
        TTo(m2, MEANt, MEANt, ALU.mult)
        var = VARt
        TTo(var, pe2, m2, ALU.subtract)
        RSQRT(var, var, 1e-5)
        for i in range(4):
            t1 = tmp()
            TTo(t1, ACCc[i], MEANt, ALU.subtract)
            TTo(t1, t1, var, ALU.mult)
            ACT(MIXc[i], t1, AF.Silu, bias=P[:, PC["lnb"] + i:PC["lnb"] + i + 1],
                scale=P[:, PC["lng"] + i:PC["lng"] + i + 1])
        if DEBUG and l == NL - 1 and dbg_state["t"] == 0:
            DBGT = T(sb("DBGT", [128, KC, TT], F32)[:])
            for c in range(KC):
                CP(DBGT[:, c, :], MIXc[c], "dve")
            S.new_dma_sem("dbg")
            DMAOUT("sp", dbg_d.rearrange("p (c t) -> p c t", c=KC), DBGT, "dbg")
        linear8(l, "out", MIXc, 8, lambda n, p: CP(Yc[n], p, "act"))
        post_norm(l, 1)

    def xattn(l):
        pre_norm(l, 2)
        linear8(l, "q", Hc, 8, lambda n, p: ACT(Qc[n], p, AF.Copy, scale=1.0 / 16.0))
        for h in range(4):
            e = E[h % 2]
            rd = RD[h % 2]
            for mc in range(2):
                p = pbig()
                for dc in range(2):
                    MM(p, KTt[l][:, 2 * h + dc, mc * 128:(mc + 1) * 128], Qc[2 * h + dc], start=(dc == 0), stop=(dc == 1))
                ACT(e[:, mc, :], p, AF.Exp)
            p = pbig()
            for mc in range(2):
                MM(p, O1b, e[:, mc, :], start=(mc == 0), stop=(mc == 1))
            S.op("dve", lambda en, o=rd.ap, i=p.ap: en.reciprocal(out=o, in_=i), p.bufs, rd.bufs)
            for dc in range(2):
                p = pbig()
                for mc in range(2):
                    MM(p, VMt[l][:, mc, (2 * h + dc) * 128:(2 * h + dc + 1) * 128], e[:, mc, :],
                       start=(mc == 0), stop=(mc == 1))
                TTo(Oc[2 * h + dc], p, rd, ALU.mult)
        linear8(l, "o", Oc, 8, lambda n, p: CP(Yc[n], p, "act"))
        post_norm(l, 3)

    def ffn(l):
        pre_norm(l, 4)
        for i in range(NFF):
            wg = wget(f"L{l}_g{i}")
            pg = pbig()
            for kc in range(KC):
                MM(pg, wg[:, kc * 128:(kc + 1) * 128], Hc[kc], start=(kc == 0), stop=(kc == KC - 1))
            wu = wget(f"L{l}_u{i}")
            pu = pbig()
            for kc in range(KC):
                MM(pu, wu[:, kc * 128:(kc + 1) * 128], Hc[kc], start=(kc == 0), stop=(kc == KC - 1))
            sg = tmp()
            ACT(sg, pg, AF.Silu)
            TTo(AVc[i], pu, sg, ALU.mult)
        for n in range(8):
            p = pbig()
            k = 0
            for part, nk in (("a", 8), ("b", 8), ("c", 6)):
                w = wget(f"L{l}_d{n}{part}")
                for kk in range(nk):
                    MM(p, w[:, kk * 128:(kk + 1) * 128], AVc[k], start=(k == 0), stop=(k == NFF - 1))
                    k += 1
            CP(Yc[n], p, "act")
        post_norm(l, 5)

    xT_v = xT.rearrange("(c p) t -> p c t", p=128)
    outT_v = outT.rearrange("(c p) t -> p c t", p=128)
    Xall = T(X[:], [b for t in Xc for b in t.bufs])
    dbg_state = {"t": 0}
    for t in range(NT):
        dbg_state["t"] = t
        DMA("sp", Xall, xT_v[:, :, t * TT:(t + 1) * TT], "xin")
        for l in range(NL):
            last = (l == NL - 1)
            mixer(l)
            if last and STAGE < 2:
                for n in range(8):
                    wget(f"L{l}_q{n}")
                for n in range(8):
                    wget(f"L{l}_o{n}")
            else:
                xattn(l)
            if last and STAGE < 3:
                for i in range(NFF):
                    wget(f"L{l}_g{i}")
                    wget(f"L{l}_u{i}")
                for n in range(8):
                    for part in "abc":
                        wget(f"L{l}_d{n}{part}")
            else:
                ffn(l)
        DMAOUT("sp", outT_v[:, :, t * TT:(t + 1) * TT], Xall, "xout")
    assert wst["use"] == len(wseq), (wst["use"], len(wseq))
    n_ins = S.emit()
    nc.sync.wait_ge(S.sem["xout"], 16 * S.dma_cnt["xout"])
    return nc, es, n_ins


_CACHE = {}


def prep_inputs(inp, NT, NL, TT):
    x = np.asarray(inp["x"], np.float32)
    mem = np.asarray(inp["mem"], np.float32)
    B = x.shape[0]
    Tn = NT * TT
    shared = {
        "memg": _vec(np.asarray(inp["mem_norm_g"], np.float32)),
        "consts": make_consts(),
        "par": np.stack([prep_params(l, inp) for l in range(NL)]),
        "lora": np.stack([prep_lora(l, inp).reshape(128, 1536) for l in range(NL)]),
    }
    wkv = []
    for l in range(NL):
        W = np.asarray(inp["wkv"][l], np.float32)
        wkv.append(np.concatenate([_unit(W, np.arange(n * 128, (n + 1) * 128), 0, 8) for n in range(16)], axis=1))
    shared["wkvu"] = np.ascontiguousarray(np.stack(wkv))
    for l in range(NL):
        shared[f"wl{l}"] = prep_layer_weights(l, inp)
    maps = []
    for b in range(B):
        m = dict(shared)
        m["xT"] = np.ascontiguousarray(x[b, :Tn].T)
        m["memT"] = np.ascontiguousarray(mem[b].T)
        maps.append(m)
    return maps


def run(inp, NT, NL, TT):
    inp = {k: np.asarray(v) for k, v in inp.items()}
    key = (NT, NL, TT)
    nc, es, n_ins = build(NT, NL, TT)
    maps = prep_inputs(inp, NT, NL, TT)
    res = run_bass_kernel_spmd(nc, maps, core_ids=list(range(len(maps))))
    out = np.stack([np.ascontiguousarray(r["outT"].T) for r in res.results])
    if DEBUG:
        global LAST_DBG
        LAST_DBG = np.stack([r["dbg"] for r in res.results])
    return out.astype(np.float32)


def kernel(**inputs):
    return run(inputs, SEQ // 256, DEPTH, 256)
```

```python
import numpy as np
import concourse.bass as bass
import concourse.mybir as mybir
from concourse.bass_utils import run_bass_kernel_spmd
from contextlib import ExitStack

F32 = mybir.dt.float32
BF16 = mybir.dt.bfloat16
AF = mybir.ActivationFunctionType
ALU = mybir.AluOpType

D = 1024
KC = 8
SEQ = 8192
DEPTH = 4
MEM = 256
DFF = 2816
NFF = DFF // 128
CW = 31
DECAY_SCALE = float(np.exp(-0.5))
NSLOT = 6
SAME_ENGINE_WINDOW = 1
CONV_ENG = "dve"
STAGE = 3
DEBUG = False


class Buf:
    __slots__ = ("w", "r")

    def __init__(self):
        self.w = None
        self.r = {}


class T:
    __slots__ = ("ap", "bufs")

    def __init__(self, ap, bufs=None):
        self.ap = ap
        if bufs is None:
            bufs = [Buf()]
        elif isinstance(bufs, Buf):
            bufs = [bufs]
        self.bufs = bufs

    def v(self, ap):
        return T(ap, self.bufs)

    def __getitem__(self, k):
        return T(self.ap[k], self.bufs)


class Sched:
    ENG = ("pe", "act", "dve", "pool", "sp")

    def __init__(self, nc, es):
        self.nc = nc
        self.es = es
        self.h = {"pe": nc.tensor, "act": nc.scalar, "dve": nc.vector, "pool": nc.gpsimd, "sp": nc.sync}
        self.ops = {k: [] for k in self.ENG}
        self.sig = {k: [] for k in self.ENG}
        self.dma_cnt = {}
        self.dma_keys = []

    def new_dma_sem(self, key):
        self.dma_cnt[key] = 0
        self.dma_keys.append(key)

    def _collect(self, reads, writes):
        deps = {}

        def add(tok):
            if tok is None:
                return
            k, v = tok
            if deps.get(k, 0) < v:
                deps[k] = v
        for b in reads:
            add(b.w)
        for b in writes:
            add(b.w)
            for k, v in b.r.items():
                add((k, v))
        return deps

    def op(self, eng, fn, reads=(), writes=(), dma=None):
        deps = self._collect(reads, writes)
        idx = len(self.ops[eng]) + 1
        fdeps = {}
        for k, v in deps.items():
            if k == eng:
                if eng == "pe" or eng == "sp" or dma is not None:
                    continue
                if v < idx - SAME_ENGINE_WINDOW:
                    continue
            fdeps[k] = v
            if k in self.sig:
                self.sig[k][v - 1] = True
        self.ops[eng].append((fn, fdeps, dma))
        self.sig[eng].append(False)
        if dma is not None:
            self.dma_cnt[dma] += 1
            tok = (dma, self.dma_cnt[dma])
        else:
            tok = (eng, idx)
        for b in reads:
            k, v = tok
            if b.r.get(k, 0) < v:
                b.r[k] = v
        for b in writes:
            b.w = tok
            b.r = {}
        return tok

    def emit(self):
        nc = self.nc
        sem = {k: self.es.enter_context(nc.semaphore("s_" + k)) for k in self.ENG}
        for k in self.dma_keys:
            sem[k] = self.es.enter_context(nc.semaphore("d_" + k))
        val = {}
        for e in self.ENG:
            c = 0
            arr = []
            for s in self.sig[e]:
                if s:
                    c += 1
                arr.append(c)
            val[e] = arr
        n_ins = 0
        for e in self.ENG:
            h = self.h[e]
            waited = {}
            for i, (fn, deps, dma) in enumerate(self.ops[e]):
                for k, v in deps.items():
                    if k in val:
                        sv = val[k][v - 1]
                    else:
                        sv = 16 * v
                    if waited.get(k, 0) >= sv:
                        continue
                    h.wait_ge(sem[k], sv)
                    waited[k] = sv
                ins = fn(h)
                n_ins += 1
                if dma is not None:
                    ins.then_inc(sem[dma], 16)
                elif self.sig[e][i]:
                    ins.then_inc(sem[e], 1)
        self.sem = sem
        return n_ins


def _unit(W, cols, k0, k1):
    sub = W[k0 * 128:k1 * 128][:, cols]
    sub = sub.reshape(k1 - k0, 128, sub.shape[1]).transpose(1, 0, 2)
    return np.ascontiguousarray(sub).reshape(128, -1)


def _vec(v):
    return np.ascontiguousarray(v.reshape(-1, 128).T)


def w_in_unit_cols(l):
    units = []
    units.append(("lo0", np.arange(2560, 2688)))
    units.append(("lo1", np.arange(2688, 2816)))
    if l > 0:
        units.append(("lo2", np.arange(2816, 2848)))
    for j in range(4):
        units.append((f"r{j}", np.arange(1024 + j * 128, 1024 + (j + 1) * 128)))
        units.append((f"k{j}", np.arange(1536 + j * 128, 1536 + (j + 1) * 128)))
        units.append((f"v{j}", np.arange(2048 + j * 128, 2048 + (j + 1) * 128)))
    for i in range(4):
        units.append((f"ca{i}", np.arange(i * 128, (i + 1) * 128)))
        units.append((f"cg{i}", np.arange(512 + i * 128, 512 + (i + 1) * 128)))
    return units


def layer_units(l):
    out = []
    for name, cols in w_in_unit_cols(l):
        out.append(("in_" + name, 8 * len(cols)))
    for n in range(8):
        out.append((f"out{n}", 1024))
    for n in range(8):
        out.append((f"q{n}", 1024))
    for n in range(8):
        out.append((f"o{n}", 1024))
    for i in range(NFF):
        out.append((f"g{i}", 1024))
        out.append((f"u{i}", 1024))
    for n in range(8):
        out.append((f"d{n}a", 1024))
        out.append((f"d{n}b", 1024))
        out.append((f"d{n}c", 6 * 128))
    return out


def prep_layer_weights(l, inp):
    w_in = inp["w_in_first"] if l == 0 else inp["w_in_rest"][l - 1]
    parts = []
    for name, cols in w_in_unit_cols(l):
        parts.append(_unit(w_in, cols, 0, 8))
    for W in (inp["w_out"][l], inp["wq"][l], inp["wo"][l]):
        for n in range(8):
            parts.append(_unit(W, np.arange(n * 128, (n + 1) * 128), 0, 8))
    wgu = inp["w_gu"][l]
    for i in range(NFF):
        parts.append(_unit(wgu, np.arange(i * 128, (i + 1) * 128), 0, 8))
        parts.append(_unit(wgu, np.arange(DFF + i * 128, DFF + (i + 1) * 128), 0, 8))
    wd = inp["w_down"][l]
    for n in range(8):
        cols = np.arange(n * 128, (n + 1) * 128)
        parts.append(_unit(wd, cols, 0, 8))
        parts.append(_unit(wd, cols, 8, 16))
        parts.append(_unit(wd, cols, 16, 22))
    return np.ascontiguousarray(np.concatenate(parts, axis=1))


PC = {}
_o = 0
for _name, _n in (("g", 48), ("mu", 15), ("cw", 124), ("cb", 4), ("lng", 4), ("lnb", 4), ("w0", 4), ("a0", 4),
                  ("v0", 4), ("kk", 4), ("ka", 4), ("rk", 4), ("xg", 4), ("xb", 4)):
    PC[_name] = _o
    _o += _n
NPAR = _o


def prep_params(l, inp):
    P = np.zeros((128, NPAR), np.float32)
    g = inp["norm_gains"][l]
    for i in range(6):
        P[:, PC["g"] + i * 8: PC["g"] + (i + 1) * 8] = _vec(g[i])
    mu = inp["mu_first"] if l == 0 else inp["mu_rest"][l - 1]
    mu_p = np.zeros(15 * 128, np.float32)
    mu_p[:mu.shape[0]] = mu
    P[:, PC["mu"]:PC["mu"] + 15] = _vec(mu_p)
    cw = inp["conv_w"][l]
    P[:, PC["cw"]:PC["cw"] + 124] = np.ascontiguousarray(
        cw.reshape(CW, 4, 128).transpose(2, 1, 0)).reshape(128, 124)
    for name, key in (("cb", "conv_b"), ("lng", "conv_ln_g"), ("lnb", "conv_ln_b"), ("w0", "w0"), ("a0", "a0"),
                      ("kk", "k_k"), ("ka", "k_a"), ("xg", "lnx_g"), ("xb", "lnx_b")):
        P[:, PC[name]:PC[name] + 4] = _vec(inp[key][l])
    P[:, PC["rk"]:PC["rk"] + 4] = _vec(inp["r_k"][l].reshape(-1))
    if l > 0:
        P[:, PC["v0"]:PC["v0"] + 4] = _vec(inp["v0"][l - 1])
    return P


def prep_lora(l, inp):
    A = np.zeros((128, 3, 512), np.float32)
    A[0:64, 0] = inp["w_up"][l]
    A[64:128, 0] = inp["a_up"][l]
    A[:, 1] = inp["g_up"][l]
    if l > 0:
        A[0:32, 2] = inp["v_up"][l - 1]
    return A


CONST_W = 1412


def make_consts():
    C = np.zeros((128, CONST_W), np.float32)
    p = np.arange(128)
    h = p // 64
    j = p % 64
    same = (h[:, None] == h[None, :])
    C[:, 0:128] = np.eye(128)
    mus = (same & (j[:, None] < j[None, :])).astype(np.float32)
    C[:, 128:256] = mus
    C[:, 256:384] = -mus
    mls = (same & (j[None, :] < j[:, None])).astype(np.float32)
    C[:, 384:512] = -mls
    t = np.arange(64)
    mui = (j[:, None] <= t[None, :]).astype(np.float32)
    C[:, 512:576] = mui
    C[:, 576:640] = -mui
    C[:, 640:768] = same.astype(np.float32) / 64.0
    C[:, 768:896] = same.astype(np.float32)
    C[:, 896:1024] = 1.0 / 1024.0
    C[:, 1024:1152] = 1.0 / 512.0
    C[:, 1152:1280] = 1.0
    C[:, 1280:1408] = -np.eye(128)
    C[:, 1408] = 1e-6
    C[:, 1409] = 1e-5
    C[:, 1410] = 64e-5
    C[:, 1411] = 1e-24
    return C


def build(NT=SEQ // 256, NL=DEPTH, TT=256):
    NCH = TT // 64
    nc = bass.Bass("TRN2", target_bir_lowering=False)
    es = ExitStack()
    S = Sched(nc, es)

    xT = nc.dram_tensor("xT", [D, NT * TT], F32, kind="ExternalInput").ap()
    memT = nc.dram_tensor("memT", [D, MEM], F32, kind="ExternalInput").ap()
    memg = nc.dram_tensor("memg", [128, 8], F32, kind="ExternalInput").ap()
    consts_d = nc.dram_tensor("consts", [128, CONST_W], F32, kind="ExternalInput").ap()
    par_d = nc.dram_tensor("par", [NL, 128, NPAR], F32, kind="ExternalInput").ap()
    lora_d = nc.dram_tensor("lora", [NL, 128, 3 * 512], F32, kind="ExternalInput").ap()
    wkv_d = nc.dram_tensor("wkvu", [NL, 128, 16 * 1024], F32, kind="ExternalInput").ap()
    lunits = [layer_units(l) for l in range(NL)]
    wl_d = [nc.dram_tensor(f"wl{l}", [128, sum(n for _, n in lunits[l])], F32, kind="ExternalInput").ap()
            for l in range(NL)]
    outT = nc.dram_tensor("outT", [D, NT * TT], F32, kind="ExternalOutput").ap()
    if DEBUG:
        dbg_d = nc.dram_tensor("dbg", [128, KC * TT], F32, kind="ExternalOutput").ap()

    def sb(name, shape, dt):
        return es.enter_context(nc.sbuf_tensor(name, shape, dt))

    def ps_t(name, shape, dt):
        return es.enter_context(nc.psum_tensor(name, shape, dt))

    X = sb("X", [128, KC, TT], F32)
    Xc = [T(X[:, c, :]) for c in range(KC)]
    Hh = sb("H", [128, KC, TT], BF16)
    Hc = [T(Hh[:, c, :]) for c in range(KC)]
    MIXb = sb("MIX", [128, KC, TT], BF16)
    MIXc = [T(MIXb[:, c, :]) for c in range(KC)]
    Y = sb("Y", [128, KC, TT], F32)
    Yc = [T(Y[:, c, :]) for c in range(KC)]
    KT = [sb(f"KT{l}", [128, KC, MEM], BF16) for l in range(NL)]
    KTt = [T(KT[l][:]) for l in range(NL)]
    VM = [sb(f"VM{l}", [128, 2, D], BF16) for l in range(NL)]
    VMt = [T(VM[l][:]) for l in range(NL)]
    RING = sb("ring", [128, NSLOT, 1024], BF16)
    ring = [T(RING[:, s, :]) for s in range(NSLOT)]
    VF = sb("VF", [128, 4, TT], F32)
    VFc = [T(VF[:, j, :]) for j in range(4)]
    HS = [[T(sb(f"HS{l}_{j}", [128, 128], F32)[:]) for j in range(4)] for l in range(NL)]
    HSb = [[[T(sb(f"HSb{l}_{j}_{k}", [128, 128], BF16)[:]) for k in range(2)] for j in range(4)] for l in range(NL)]
    hsb_par = [[0] * 4 for _ in range(NL)]
    PAR = [T(sb(f"PAR{l}", [128, NPAR], F32)[:]) for l in range(NL)]
    LORA = [T(sb(f"LORA{l}", [128, 3, 512], BF16)[:]) for l in range(NL)]
    CH = [T(sb(f"CH{l}", [128, 4, CW - 1], F32)[:]) for l in range(NL)]
    SH = [T(sb(f"SH{l}", [128, 15], F32)[:]) for l in range(NL)]
    CF = T(sb("CF", [128, CONST_W], F32)[:])
    CB = T(sb("CB", [128, CONST_W], BF16)[:])

    def cf(a, b):
        return CF[:, a:b]

    def cb(a, b):
        return CB[:, a:b]
    IDb = cb(0, 128)
    MUS, NMUS, NMLS = cf(128, 256), cf(256, 384), cf(384, 512)
    MUI, NMUI = cf(512, 576), cf(576, 640)
    IDf = cf(0, 128)
    BO64b, BO1b = cb(640, 768), cb(768, 896)
    O1024b, O512b, O1b = cb(896, 1024), cb(1024, 1152), cb(1152, 1280)

    RSTD = T(sb("RSTD", [128, TT], F32)[:])
    SQ = [T(sb(f"SQ{i}", [128, TT], BF16)[:]) for i in range(2)]
    sq_i = [0]
    PT = [T(sb(f"PT{i}", [128, TT + 1], F32)[:]) for i in range(3)]
    pt_i = [0]
    TMP = [T(sb(f"TMP{i}", [128, TT], F32)[:]) for i in range(4)]
    tmp_i = [0]

    def tmp():
        t = TMP[tmp_i[0] % len(TMP)]
        tmp_i[0] += 1
        return t
    LOb = T(sb("LOb", [128, TT], BF16)[:])
    Gb = T(sb("Gb", [128, TT], BF16)[:])
    VRb = T(sb("VRb", [32, TT], BF16)[:])
    RAW = [[T(sb(f"RAW{i}_{q}", [128, TT], F32)[:]) for q in range(3)] for i in range(2)]
    RAW.append([Yc[0], Yc[1], Yc[2]])
    RAW.append([Yc[3], Yc[4], Yc[5]])
    P0 = sb("P0", [128, NCH, 96], F32)
    P1 = sb("P1", [128, NCH, 96], F32)
    P0t, P1t = T(P0[:]), T(P1[:])
    At = T(sb("A_t", [128, TT], F32)[:])
    KKt = T(sb("KK_t", [128, TT], F32)[:])
    Kpt = T(sb("Kp_t", [128, TT], F32)[:])
    Bvt = T(sb("Bv_t", [128, TT], F32)[:])
    EX = [T(sb(f"EX{i}", [128, NCH, 64], F32)[:]) for i in range(4)]
    GCt = [T(sb(f"GC{i}", [128, NCH], F32)[:]) for i in range(2)]
    NSET = 2
    EXPN = ("KKe", "KTe", "BTe", "KHe", "BHe", "Ve")
    EXP = [{n: T(sb(f"{n}{i}", [128, NCH, 128], BF16)[:]) for n in EXPN} for i in range(NSET)]
    RT = [T(sb(f"Rt{i}", [128, TT], BF16)[:]) for i in range(NSET)]
    Gt = [T(sb(f"G_t{i}", [128, TT], F32)[:]) for i in range(NSET)]
    BON = [T(sb(f"BON{i}", [128, TT], F32)[:]) for i in range(NSET)]
    YR = [T(sb(f"YR{i}", [128, TT], F32)[:]) for i in range(NSET)]
    MATN = ("LkkT", "M1", "N1", "Sa", "Sb", "Ma", "Mb", "Na", "Nb", "Vbd", "KHbd", "BHbdn", "KKbd", "X1", "TKT", "Usb")
    NMSET = NCH
    MATS = []
    for i in range(min(2, NMSET)):
        d_ = {n: T(sb(f"m_{n}{i}", [128, 128], BF16)[:]) for n in MATN}
        d_["MrkT"] = T(sb(f"m_MrkT{i}", [128, 64], BF16)[:])
        d_["MrbTn"] = T(sb(f"m_MrbTn{i}", [128, 64], BF16)[:])
        MATS.append(d_)
    mset_i = [0]
    HG = sb("HG", [128, 4, CW - 1 + TT], F32)
    HGc = [T(HG[:, i, :]) for i in range(4)]
    ACC = sb("ACC", [128, 4, TT], F32)
    ACCc = [T(ACC[:, i, :]) for i in range(4)]
    MEANt = T(sb("MEANt", [128, TT], F32)[:])
    VARt = T(sb("VARt", [128, TT], F32)[:])
    Q = sb("Q", [128, KC, TT], BF16)
    Qc = [T(Q[:, c, :]) for c in range(KC)]
    E = [T(sb(f"E{i}", [128, 2, TT], BF16)[:]) for i in range(2)]
    RD = [T(sb(f"RD{i}", [128, TT], F32)[:]) for i in range(2)]
    Oo = sb("Oo", [128, KC, TT], BF16)
    Oc = [T(Oo[:, c, :]) for c in range(KC)]
    AV = sb("AV", [128, NFF, TT], BF16)
    AVc = [T(AV[:, i, :], [Buf() for _ in range(TT // 128)]) for i in range(NFF)]
    _slots = [(i, k) for i in range(NFF) for k in range(TT // 128)]
    _si = 0
    for i in range(2, NMSET):
        d_ = {}
        for n in list(MATN) + ["MrkT", "MrbTn"]:
            ci, k = _slots[_si]
            _si += 1
            wdt = 64 if n in ("MrkT", "MrbTn") else 128
            d_[n] = T(AV[:, ci, k * 128:k * 128 + wdt], [AVc[ci].bufs[k]])
        MATS.append(d_)
    MN = T(Hh[:, :, 0:MEM], [b for t in Hc for b in t.bufs])
    MF = T(Y[:, :, 0:MEM], [b for t in Yc for b in t.bufs])
    MG = T(sb("MG", [128, 8], F32)[:])
    VTt = T(sb("VTt", [128, MEM], BF16)[:])

    PSB = [ps_t(f"psb{i}", [128, 512], F32) for i in range(8)]
    PSBT = [T(PSB[b][:]) for b in range(8)]
    pb_i = [0]

    def pbig():
        t = PSBT[pb_i[0] % 8]
        pb_i[0] += 1
        return t[:, 0:TT]

    def psm():
        t = PSBT[pb_i[0] % 8]
        pb_i[0] += 1
        return t[:, 0:128]

    def bufs_of(*ts):
        out = []
        for t in ts:
            if isinstance(t, T):
                out.extend(t.bufs)
        return out

    def apof(x):
        return x.ap if isinstance(x, T) else x

    def MM(out, lhsT, rhs, start=True, stop=True):
        S.op("pe", lambda e: e.matmul(out.ap, lhsT=lhsT.ap, rhs=rhs.ap, start=start, stop=stop),
             bufs_of(lhsT, rhs), out.bufs)

    def ACT(out, in_, func, bias=None, scale=1.0):
        kw = {}
        if bias is not None:
            kw["bias"] = apof(bias)
        S.op("act", lambda e: e.activation(out=out.ap, in_=in_.ap, func=func, scale=apof(scale), **kw),
             bufs_of(in_, bias, scale), out.bufs)

    def TTo(out, in0, in1, op, eng="dve"):
        S.op(eng, lambda e: e.tensor_tensor(out=out.ap, in0=in0.ap, in1=in1.ap, op=op),
             bufs_of(in0, in1), out.bufs)

    def TS(out, in0, s1, op0, s2=None, op1=None, eng="dve"):
        if op1 is None:
            S.op(eng, lambda e: e.tensor_scalar(out=out.ap, in0=in0.ap, scalar1=apof(s1), scalar2=None, op0=op0),
                 bufs_of(in0, s1), out.bufs)
        else:
            S.op(eng, lambda e: e.tensor_scalar(out=out.ap, in0=in0.ap, scalar1=apof(s1), scalar2=apof(s2),
                                                op0=op0, op1=op1),
                 bufs_of(in0, s1, s2), out.bufs)

    def STT(out, in0, sc, in1, op0, op1, eng="dve"):
        S.op(eng, lambda e: e.scalar_tensor_tensor(out=out.ap, in0=in0.ap, scalar=apof(sc), in1=in1.ap,
                                                   op0=op0, op1=op1),
             bufs_of(in0, sc, in1), out.bufs)

    def CP(out, in_, eng="dve"):
        if eng == "act":
            ACT(out, in_, AF.Copy)
        else:
            S.op(eng, lambda e: e.tensor_copy(out=out.ap, in_=in_.ap), in_.bufs, out.bufs)

    def MEMSET(t, val, eng="dve"):
        S.op(eng, lambda e: e.memset(t.ap, val), (), t.bufs)

    def DMA(eng, out, in_ap, key):
        S.op(eng, lambda e: e.dma_start(out=out.ap, in_=in_ap), (), out.bufs, dma=key)

    def DMAOUT(eng, out_ap, in_, key):
        S.op(eng, lambda e: e.dma_start(out=out_ap, in_=in_.ap), in_.bufs, (), dma=key)

    EPSC = {1e-6: 1408, 1e-5: 1409, 64e-5: 1410, 1e-24: 1411}

    def RSQRT(out, in_, eps):
        c = EPSC[eps]
        ACT(out, in_, AF.Sqrt, bias=CF[:, c:c + 1])
        S.op("dve", lambda e: e.reciprocal(out=out.ap, in_=out.ap), out.bufs, out.bufs)

    evac_i = [0]

    def EVAC(out, in_):
        evac_i[0] += 1
        if evac_i[0] % 3:
            CP(out, in_, "act")
        else:
            CP(out, in_, "dve")

    for s in range(NSLOT):
        S.new_dma_sem(f"w{s}")
    wseq = []
    for l in range(NL):
        for n in range(16):
            wseq.append((wkv_d[l, :, n * 1024:(n + 1) * 1024], 1024, f"kv{l}_{n}"))
    for t in range(NT):
        for l in range(NL):
            off = 0
            for name, ncols in lunits[l]:
                wseq.append((wl_d[l][:, off:off + ncols], ncols, f"L{l}_{name}"))
                off += ncols
    wst = {"issue": 0, "use": 0}

    def wget(expect):
        while wst["issue"] < min(len(wseq), wst["use"] + NSLOT):
            u = wst["issue"]
            ap, ncols, _ = wseq[u]
            slot = u % NSLOT
            DMA("pool", ring[slot][:, 0:ncols], ap, f"w{slot}")
            wst["issue"] += 1
        u = wst["use"]
        assert wseq[u][2] == expect, (wseq[u][2], expect)
        wst["use"] += 1
        return ring[u % NSLOT]

    def setup_dma(eng, out, in_ap, key):
        S.new_dma_sem(key)
        DMA(eng, out, in_ap, key)
    for k in ("xin", "xout"):
        S.new_dma_sem(k)
    setup_dma("sp", CF, consts_d[:, :], "cst")
    setup_dma("pool", CB, consts_d[:, :], "cstb")
    for l in range(NL):
        setup_dma("sp", PAR[l], par_d[l], f"par{l}")
    for l in range(NL):
        setup_dma("pool", LORA[l], lora_d[l].rearrange("p (a b) -> p a b", a=3), f"lora{l}")
    setup_dma("sp", MF, memT.rearrange("(c p) m -> p c m", p=128), "mem")
    setup_dma("sp", MG, memg[:, :], "memg")
    for l in range(NL):
        for j in range(4):
            MEMSET(HS[l][j], 0.0)
            MEMSET(HSb[l][j][0], 0.0)
            MEMSET(HSb[l][j][1], 0.0)
        MEMSET(CH[l], 0.0)
        MEMSET(SH[l], 0.0)
    for i in range(NSET):
        for n in EXPN:
            MEMSET(EXP[i][n], 0.0)
    MEMSET(P0t, 0.0)
    MEMSET(P1t, 0.0)

    def sqtile():
        t = SQ[sq_i[0] % 2]
        sq_i[0] += 1
        return t

    def rms_stats(src_list, out_rstd, ncol, ones_b, eps):
        p = pbig()
        n = len(src_list)
        for i, s in enumerate(src_list):
            q = sqtile()
            ACT(q[:, 0:ncol], s, AF.Square)
            MM(p[:, 0:ncol], ones_b, q[:, 0:ncol], start=(i == 0), stop=(i == n - 1))
        RSQRT(out_rstd, p[:, 0:ncol], eps)

    MFc = [MF[:, c, :] for c in range(KC)]
    MRS = T(sb("MRS", [128, MEM], F32)[:])
    rms_stats(MFc, MRS, MEM, O1024b, 1e-6)
    for c in range(KC):
        STT(MN[:, c, :], MF[:, c, :], MG[:, c:c + 1], MRS, ALU.mult, ALU.mult)
    for l in range(NL):
        for n in range(16):
            w = wget(f"kv{l}_{n}")
            p = pbig()
            for kc in range(KC):
                MM(p[:, 0:MEM], w[:, kc * 128:(kc + 1) * 128], MN[:, kc, :], start=(kc == 0), stop=(kc == KC - 1))
            if n < 8:
                EVAC(KTt[l][:, n, :], p[:, 0:MEM])
            else:
                EVAC(VTt, p[:, 0:MEM])
                for mc in range(2):
                    q = psm()
                    MM(q, VTt[:, mc * 128:(mc + 1) * 128], IDb)
                    EVAC(VMt[l][:, mc, (n - 8) * 128:(n - 7) * 128], q)

    def pre_norm(l, gi):
        rms_stats(Xc, RSTD, TT, O1024b, 1e-6)
        for c in range(KC):
            col = PC["g"] + gi * 8 + c
            STT(Hc[c], Xc[c], PAR[l][:, col:col + 1], RSTD, ALU.mult, ALU.mult)

    def post_norm(l, gi):
        rms_stats(Yc, RSTD, TT, O1024b, 1e-6)
        for c in range(KC):
            col = PC["g"] + gi * 8 + c
            t = tmp()
            STT(t, Yc[c], PAR[l][:, col:col + 1], RSTD, ALU.mult, ALU.mult)
            TTo(Xc[c], Xc[c], t, ALU.add)

    def linear8(l, prefix, src, nout, consume):
        for n in range(nout):
            w = wget(f"L{l}_{prefix}{n}")
            p = pbig()
            for kc in range(KC):
                MM(p, w[:, kc * 128:(kc + 1) * 128], src[kc], start=(kc == 0), stop=(kc == KC - 1))
            consume(n, p)

    def rwkv_pre(l, j, raw, si):
        R, K, V = raw
        P = PAR[l]
        ex = EXP[si]
        pc = lambda name: P[:, PC[name] + j:PC[name] + j + 1]
        lora = LORA[l]
        jc = slice(j * 128, (j + 1) * 128)
        v3 = lambda t: t.v(t.ap.rearrange("p (c t) -> p c t", t=64))
        p = pbig()
        MM(p, lora[0:64, 0, jc], LOb[0:64, :])
        t1 = tmp()
        ACT(t1, p, AF.Sigmoid, bias=pc("w0"))
        LG = P0t[:, :, 32:96]
        TS(LG, v3(t1), -DECAY_SCALE, ALU.mult)
        yield
        p = pbig()
        MM(p, lora[64:128, 0, jc], LOb[64:128, :])
        ACT(At, p, AF.Sigmoid, bias=pc("a0"))
        p = pbig()
        MM(p, lora[:, 1, jc], Gb)
        CP(Gt[si], p, "act")
        yield
        if l == 0:
            CP(VFc[j], V, "dve")
        else:
            p = pbig()
            MM(p, lora[0:32, 2, jc], VRb[0:32, :])
            t2 = tmp()
            ACT(t2, p, AF.Sigmoid, bias=pc("v0"))
            t3 = tmp()
            TTo(t3, VFc[j], V, ALU.subtract)
            TTo(t3, t3, t2, ALU.mult)
            TTo(V, V, t3, ALU.add)
        yield
        TS(KKt, K, pc("kk"), ALU.mult)
        q = sqtile()
        ACT(q, KKt, AF.Square)
        p = pbig()
        MM(p, BO1b, q)
        t4 = tmp()
        RSQRT(t4, p, 1e-24)
        TTo(KKt, KKt, t4, ALU.mult)
        yield
        t5 = tmp()
        TS(t5, At, pc("ka"), ALU.mult, pc("ka"), ALU.subtract)
        STT(Kpt, t5, 1.0, K, ALU.add, ALU.mult)
        TTo(Bvt, KKt, At, ALU.mult)
        yield
        q = sqtile()
        STT(q, R, pc("rk"), Kpt, ALU.mult, ALU.mult)
        p = pbig()
        MM(p, BO1b, q)
        TTo(BON[si], p, V, ALU.mult)
        yield
        src, dst = P0t, P1t
        for s_ in (1, 2, 4, 8, 16, 32):
            TTo(dst[:, :, 32:96], src[:, :, 32:96], src[:, :, 32 - s_:96 - s_], ALU.add)
            src, dst = dst, src
            yield
        assert src is P0t
        LGP = P0t[:, :, 31:95]
        ACT(EX[0], LG, AF.Exp)
        ACT(EX[1], LGP, AF.Exp)
        ACT(EX[2], LG, AF.Exp, scale=-1.0)
        gc = GCt[si]
        ACT(gc, P0t[:, :, 95], AF.Exp)
        yield
        TTo(EX[3], EX[2], gc.v(gc.ap.unsqueeze(2).to_broadcast([128, NCH, 64])), ALU.mult)
        TTo(RT[si], R, EX[0].v(EX[0].ap.rearrange("p c t -> p (c t)")), ALU.mult)
        yield
        for h in range(2):
            ph = slice(h * 64, (h + 1) * 64)
            fh = slice(h * 64, (h + 1) * 64)
            TTo(ex["KKe"][ph, :, fh], v3(KKt)[ph], EX[1][ph], ALU.mult)
            TTo(ex["KTe"][ph, :, fh], v3(Kpt)[ph], EX[2][ph], ALU.mult)
            yield
            TTo(ex["BTe"][ph, :, fh], v3(Bvt)[ph], EX[2][ph], ALU.mult)
            TTo(ex["KHe"][ph, :, fh], v3(Kpt)[ph], EX[3][ph], ALU.mult)
            yield
            TTo(ex["BHe"][ph, :, fh], v3(Bvt)[ph], EX[3][ph], ALU.mult)
            CP(ex["Ve"][ph, :, fh], v3(V)[ph], "dve")
            yield

    def rwkv_chunks(l, j, si):
        ex = EXP[si]
        ms = [MATS[c % NMSET] for c in range(NCH)]
        assert NMSET >= NCH
        opnd = []
        for c in range(NCH):
            opnd.append([ex[n][:, c, :] for n in EXPN] + [RT[si][:, c * 64:(c + 1) * 64]])
        for c in range(NCH):
            m = ms[c]
            KKe, KTe, BTe, KHe, BHe, Ve, Rc = opnd[c]
            p = psm(); MM(p, KTe, KKe); TTo(m["LkkT"], p, MUS, ALU.mult)
            p = psm(); MM(p, BTe, KKe); TTo(m["M1"], p, NMUS, ALU.mult)
            p = psm(); MM(p, KKe, BTe); TTo(m["N1"], p, NMLS, ALU.mult)
            yield
            p = psm(); MM(p[:, 0:64], KTe, Rc); TTo(m["MrkT"], p[:, 0:64], MUI, ALU.mult)
            p = psm(); MM(p[:, 0:64], BTe, Rc); TTo(m["MrbTn"], p[:, 0:64], NMUI, ALU.mult)
            TTo(m["Sa"], m["M1"], IDf, ALU.add)
            yield
            p = psm(); MM(p, Ve, IDb); EVAC(m["Vbd"], p)
            p = psm(); MM(p, KHe, IDb); EVAC(m["KHbd"], p)
            yield
            p = psm(); MM(p, BHe, cb(1280, 1408)); EVAC(m["BHbdn"], p)
            p = psm(); MM(p, KKe, IDb); EVAC(m["KKbd"], p)
            yield
        cur = [[ms[c]["M1"], ms[c]["N1"], ms[c]["Sa"], ms[c]["Ma"], ms[c]["Na"], ms[c]["Sb"]] for c in range(NCH)]
        for lvl in range(5):
            last = (lvl == 4)
            for c in range(NCH):
                m = ms[c]
                Mc, Nc, Sc, Mn, Nn, Sn = cur[c]
                if not last:
                    p = psm(); MM(p, Nc, Mc); EVAC(Mn, p)
                p = psm(); MM(p, Mc, Nc); EVAC(Nn, p)
                yield
            for c in range(NCH):
                m = ms[c]
                Mc, Nc, Sc, Mn, Nn, Sn = cur[c]
                p = psm(); MM(p, Nn, Sc); TTo(Sn, p, Sc, ALU.add)
                if lvl == 0:
                    cur[c] = [Mn, Nn, Sn, m["Mb"], m["Nb"], m["Sa"]]
                else:
                    cur[c] = [Mn, Nn, Sn, Mc, Nc, Sc]
                yield
        for c in range(NCH):
            m = ms[c]
            TT_ = cur[c][2]
            p = psm(); MM(p, m["LkkT"], m["Vbd"]); EVAC(m["X1"], p)
            p = psm(); MM(p, m["KKbd"], TT_); EVAC(m["TKT"], p)
            yield
        hs = HS[l][j]
        for c in range(NCH):
            m = ms[c]
            TT_ = cur[c][2]
            Rc = opnd[c][6]
            gcol = GCt[si][:, c:c + 1]
            hb_old = HSb[l][j][hsb_par[l][j]]
            hb_new = HSb[l][j][1 - hsb_par[l][j]]
            hsb_par[l][j] = 1 - hsb_par[l][j]
            p = psm()
            MM(p, TT_, m["X1"], start=True, stop=False)
            MM(p, m["TKT"], hb_old, start=False, stop=True)
            CP(m["Usb"], p, "act")
            p = psm()
            MM(p, m["KHbd"], m["Vbd"], start=True, stop=False)
            MM(p, m["BHbdn"], m["Usb"], start=False, stop=True)
            STT(hs, hs, gcol, p, ALU.mult, ALU.add)
            CP(hb_new, hs, "act")
            py = psm()
            MM(py[:, 0:64], hb_old, Rc, start=True, stop=False)
            MM(py[:, 0:64], m["Vbd"], m["MrkT"], start=False, stop=False)
            MM(py[:, 0:64], m["Usb"], m["MrbTn"], start=False, stop=True)
            CP(YR[si][:, c * 64:(c + 1) * 64], py[:, 0:64], "act")
            yield

    def rwkv_post(l, j, si):
        P = PAR[l]
        pc = lambda name: P[:, PC[name] + j:PC[name] + j + 1]
        yr = YR[si]
        q = sqtile()
        CP(q, yr, "act")
        pm = pbig()
        MM(pm, BO64b, q)
        q2 = sqtile()
        ACT(q2, yr, AF.Square)
        pe2 = pbig()
        MM(pe2, BO64b, q2)
        mean = tmp()
        CP(mean, pm, "act")
        m2 = tmp()
        TTo(m2, mean, mean, ALU.mult)
        var = tmp()
        TTo(var, pe2, m2, ALU.subtract)
        RSQRT(var, var, 64e-5)
        t1 = tmp()
        TTo(t1, yr, mean, ALU.subtract)
        TTo(t1, t1, var, ALU.mult)
        TS(t1, t1, pc("xg"), ALU.mult, pc("xb"), ALU.add)
        TTo(t1, t1, BON[si], ALU.add)
        TTo(MIXc[4 + j], t1, Gt[si], ALU.mult)

    def conv_task(l):
        P = PAR[l]
        for jj in range(CW):
            for i in range(4):
                wcol = P[:, PC["cw"] + i * CW + jj:PC["cw"] + i * CW + jj + 1]
                if jj == 0:
                    TS(ACCc[i], HGc[i][:, 0:TT], wcol, ALU.mult, P[:, PC["cb"] + i:PC["cb"] + i + 1], ALU.add,
                       eng=CONV_ENG)
                else:
                    STT(ACCc[i], HGc[i][:, jj:jj + TT], wcol, ACCc[i], ALU.mult, ALU.add, eng=CONV_ENG)
            yield
        pm = pbig()
        pe2 = pbig()
        for i in range(4):
            q = sqtile()
            CP(q, ACCc[i], "act")
            MM(pm, O512b, q, start=(i == 0), stop=(i == 3))
            q2 = sqtile()
            ACT(q2, ACCc[i], AF.Square)
            MM(pe2, O512b, q2, start=(i == 0), stop=(i == 3))
        CP(MEANt, pm, "act")
        m2 = tmp()
        TTo(m2, MEANt, MEANt, ALU.mult)
        var = VARt
        TTo(var, pe2, m2, ALU.subtract)
        RSQRT(var, var, 1e-5)
        yield
        for i in range(4):
            t1 = tmp()
            TTo(t1, ACCc[i], MEANt, ALU.subtract)
            TTo(t1, t1, var, ALU.mult)
            ACT(MIXc[i], t1, AF.Silu, bias=P[:, PC["lnb"] + i:PC["lnb"] + i + 1],
                scale=P[:, PC["lng"] + i:PC["lng"] + i + 1])
            yield

    def mixer(l):
        P = PAR[l]
        pre_norm(l, 0)
        for i in range(4):
            CP(HGc[i][:, 0:CW - 1], CH[l][:, i, :], "act")
        names = [n for n, _ in w_in_unit_cols(l)]
        state = {}
        for name in names:
            w = wget(f"L{l}_in_{name}")
            ncol = 32 if name == "lo2" else 128
            p = pbig()
            for kc in range(KC):
                MM(p[0:ncol, :], w[:, kc * ncol:(kc + 1) * ncol], Hc[kc], start=(kc == 0), stop=(kc == KC - 1))
            kind = name[0]
            if kind in ("l", "r", "k", "v"):
                if kind == "l":
                    midx = 12 + int(name[2])
                else:
                    midx = {"r": 0, "k": 4, "v": 8}[kind] + int(name[1])
                pt = PT[pt_i[0] % len(PT)]
                pt_i[0] += 1
                rows = slice(0, ncol)
                CP(pt[rows, 0:1], SH[l][rows, midx:midx + 1], "act")
                CP(pt[rows, 1:TT + 1], p[rows, :], "act")
                d = tmp()
                TTo(d[rows], pt[rows, 0:TT], pt[rows, 1:TT + 1], ALU.subtract)
                CP(SH[l][rows, midx:midx + 1], pt[rows, TT:TT + 1], "act")
                mucol = P[rows, PC["mu"] + midx:PC["mu"] + midx + 1]
                if kind == "l":
                    sh = tmp()
                    STT(sh[rows], d[rows], mucol, pt[rows, 1:TT + 1], ALU.mult, ALU.add)
                    if name == "lo0":
                        ACT(LOb[0:64, :], sh[0:64, :], AF.Tanh)
                        CP(LOb[64:128, :], sh[64:128, :], "act")
                    elif name == "lo1":
                        ACT(Gb, sh, AF.Sigmoid)
                    else:
                        CP(VRb[0:32, :], sh[0:32, :], "act")
                else:
                    j = int(name[1])
                    dst = RAW[j][{"r": 0, "k": 1, "v": 2}[kind]]
                    STT(dst, d, mucol, pt[:, 1:TT + 1], ALU.mult, ALU.add)
            else:
                i = int(name[2])
                if name[1] == "a":
                    state["pa"] = p
                else:
                    sg = tmp()
                    ACT(sg, p, AF.Sigmoid)
                    TTo(HGc[i][:, CW - 1:CW - 1 + TT], state["pa"], sg, ALU.mult)
        for i in range(4):
            CP(CH[l][:, i, :], HGc[i][:, TT:TT + CW - 1], "act")
        prog = {"pre": 0, "chunks": 0}

        def thread_pre():
            for j in range(4):
                while prog["chunks"] < j - NSET + 1:
                    yield
                yield from rwkv_pre(l, j, RAW[j], j % NSET)
                prog["pre"] = j + 1
                yield

        def thread_chunks():
            for j in range(4):
                while prog["pre"] < j + 1:
                    yield
                yield from rwkv_chunks(l, j, j % NSET)
                prog["chunks"] = j + 1
                rwkv_post(l, j, j % NSET)
                yield

        active = [thread_pre(), thread_chunks(), conv_task(l)]
        while active:
            for g in list(active):
                try:
                    next(g)
                except StopIteration:
                    active.remove(g)
        if DEBUG and l == NL - 1 and dbg_state["t"] == 0:
            DBGT = T(sb("DBGT", [128, KC, TT], F32)[:])
            for c in range(KC):
                CP(DBGT[:, c, :], MIXc[c], "dve")
            S.new_dma_sem("dbg")
            DMAOUT("sp", dbg_d.rearrange("p (c t) -> p c t", c=KC), DBGT, "dbg")
        linear8(l, "out", MIXc, 8, lambda n, p: CP(Yc[n], p, "act"))
        post_norm(l, 1)

    def xattn(l):
        pre_norm(l, 2)
        linear8(l, "q", Hc, 8, lambda n, p: ACT(Qc[n], p, AF.Copy, scale=1.0 / 16.0))
        for h in range(4):
            e = E[h % 2]
            rd = RD[h % 2]
            for mc in range(2):
                p = pbig()
                for dc in range(2):
                    MM(p, KTt[l][:, 2 * h + dc, mc * 128:(mc + 1) * 128], Qc[2 * h + dc], start=(dc == 0), stop=(dc == 1))
                ACT(e[:, mc, :], p, AF.Exp)
            p = pbig()
            for mc in range(2):
                MM(p, O1b, e[:, mc, :], start=(mc == 0), stop=(mc == 1))
            S.op("dve", lambda en, o=rd.ap, i=p.ap: en.reciprocal(out=o, in_=i), p.bufs, rd.bufs)
            for dc in range(2):
                p = pbig()
                for mc in range(2):
                    MM(p, VMt[l][:, mc, (2 * h + dc) * 128:(2 * h + dc + 1) * 128], e[:, mc, :],
                       start=(mc == 0), stop=(mc == 1))
                TTo(Oc[2 * h + dc], p, rd, ALU.mult)
        linear8(l, "o", Oc, 8, lambda n, p: CP(Yc[n], p, "act"))
        post_norm(l, 3)

    def ffn(l):
        pre_norm(l, 4)
        for i in range(NFF):
            wg = wget(f"L{l}_g{i}")
            pg = pbig()
            for kc in range(KC):
                MM(pg, wg[:, kc * 128:(kc + 1) * 128], Hc[kc], start=(kc == 0), stop=(kc == KC - 1))
            wu = wget(f"L{l}_u{i}")
            pu = pbig()
            for kc in range(KC):
                MM(pu, wu[:, kc * 128:(kc + 1) * 128], Hc[kc], start=(kc == 0), stop=(kc == KC - 1))
            sg = tmp()
            ACT(sg, pg, AF.Silu)
            TTo(AVc[i], pu, sg, ALU.mult)
        for n in range(8):
            p = pbig()
            k = 0
            for part, nk in (("a", 8), ("b", 8), ("c", 6)):
                w = wget(f"L{l}_d{n}{part}")
                for kk in range(nk):
                    MM(p, w[:, kk * 128:(kk + 1) * 128], AVc[k], start=(k == 0), stop=(k == NFF - 1))
                    k += 1
            CP(Yc[n], p, "act")
        post_norm(l, 5)

    xT_v = xT.rearrange("(c p) t -> p c t", p=128)
    outT_v = outT.rearrange("(c p) t -> p c t", p=128)
    Xall = T(X[:], [b for t in Xc for b in t.bufs])
    dbg_state = {"t": 0}
    for t in range(NT):
        dbg_state["t"] = t
        DMA("sp", Xall, xT_v[:, :, t * TT:(t + 1) * TT], "xin")
        for l in range(NL):
            last = (l == NL - 1)
            mixer(l)
            if last and STAGE < 2:
                for n in range(8):
                    wget(f"L{l}_q{n}")
                for n in range(8):
                    wget(f"L{l}_o{n}")
            else:
                xattn(l)
            if last and STAGE < 3:
                for i in range(NFF):
                    wget(f"L{l}_g{i}")
                    wget(f"L{l}_u{i}")
                for n in range(8):
                    for part in "abc":
                        wget(f"L{l}_d{n}{part}")
            else:
                ffn(l)
        DMAOUT("sp", outT_v[:, :, t * TT:(t + 1) * TT], Xall, "xout")
    assert wst["use"] == len(wseq), (wst["use"], len(wseq))
    n_ins = S.emit()
    nc.sync.wait_ge(S.sem["xout"], 16 * S.dma_cnt["xout"])
    return nc, es, n_ins


_CACHE = {}


def prep_inputs(inp, NT, NL, TT):
    x = np.asarray(inp["x"], np.float32)
    mem = np.asarray(inp["mem"], np.float32)
    B = x.shape[0]
    Tn = NT * TT
    shared = {
        "memg": _vec(np.asarray(inp["mem_norm_g"], np.float32)),
        "consts": make_consts(),
        "par": np.stack([prep_params(l, inp) for l in range(NL)]),
        "lora": np.stack([prep_lora(l, inp).reshape(128, 1536) for l in range(NL)]),
    }
    wkv = []
    for l in range(NL):
        W = np.asarray(inp["wkv"][l], np.float32)
        wkv.append(np.concatenate([_unit(W, np.arange(n * 128, (n + 1) * 128), 0, 8) for n in range(16)], axis=1))
    shared["wkvu"] = np.ascontiguousarray(np.stack(wkv))
    for l in range(NL):
        shared[f"wl{l}"] = prep_layer_weights(l, inp)
    maps = []
    for b in range(B):
        m = dict(shared)
        m["xT"] = np.ascontiguousarray(x[b, :Tn].T)
        m["memT"] = np.ascontiguousarray(mem[b].T)
        maps.append(m)
    return maps


def run(inp, NT, NL, TT):
    inp = {k: np.asarray(v) for k, v in inp.items()}
    key = (NT, NL, TT)
    nc, es, n_ins = build(NT, NL, TT)
    maps = prep_inputs(inp, NT, NL, TT)
    res = run_bass_kernel_spmd(nc, maps, core_ids=list(range(len(maps))))
    out = np.stack([np.ascontiguousarray(r["outT"].T) for r in res.results])
    if DEBUG:
        global LAST_DBG
        LAST_DBG = np.stack([r["dbg"] for r in res.results])
    return out.astype(np.float32)


def kernel(**inputs):
    return run(inputs, SEQ // 256, DEPTH, 256)
```

```python
import numpy as np
import concourse.bass as bass
import concourse.mybir as mybir
from concourse.bass_utils import run_bass_kernel_spmd
from contextlib import ExitStack

F32 = mybir.dt.float32
BF16 = mybir.dt.bfloat16
AF = mybir.ActivationFunctionType
ALU = mybir.AluOpType

D = 1024
KC = 8
SEQ = 8192
DEPTH = 4
MEM = 256
DFF = 2816
NFF = DFF // 128
CW = 31
DECAY_SCALE = float(np.exp(-0.5))
NSLOT = 6
SAME_ENGINE_WINDOW = 1
CONV_ENG = "dve"
STAGE = 3
DEBUG = False


class Buf:
    __slots__ = ("w", "r")

    def __init__(self):
        self.w = None
        self.r = {}


class T:
    __slots__ = ("ap", "bufs")

    def __init__(self, ap, bufs=None):
        self.ap = ap
        if bufs is None:
            bufs = [Buf()]
        elif isinstance(bufs, Buf):
            bufs = [bufs]
        self.bufs = bufs

    def v(self, ap):
        return T(ap, self.bufs)

    def __getitem__(self, k):
        return T(self.ap[k], self.bufs)


class Sched:
    ENG = ("pe", "act", "dve", "pool", "sp")

    def __init__(self, nc, es):
        self.nc = nc
        self.es = es
        self.h = {"pe": nc.tensor, "act": nc.scalar, "dve": nc.vector, "pool": nc.gpsimd, "sp": nc.sync}
        self.ops = {k: [] for k in self.ENG}
        self.sig = {k: [] for k in self.ENG}
        self.dma_cnt = {}
        self.dma_keys = []

    def new_dma_sem(self, key):
        self.dma_cnt[key] = 0
        self.dma_keys.append(key)

    def _collect(self, reads, writes):
        deps = {}

        def add(tok):
            if tok is None:
                return
            k, v = tok
            if deps.get(k, 0) < v:
                deps[k] = v
        for b in reads:
            add(b.w)
        for b in writes:
            add(b.w)
            for k, v in b.r.items():
                add((k, v))
        return deps

    def op(self, eng, fn, reads=(), writes=(), dma=None):
        deps = self._collect(reads, writes)
        idx = len(self.ops[eng]) + 1
        fdeps = {}
        for k, v in deps.items():
            if k == eng:
                if eng == "pe" or eng == "sp" or dma is not None:
                    continue
                if v < idx - SAME_ENGINE_WINDOW:
                    continue
            fdeps[k] = v
            if k in self.sig:
                self.sig[k][v - 1] = True
        self.ops[eng].append((fn, fdeps, dma))
        self.sig[eng].append(False)
        if dma is not None:
            self.dma_cnt[dma] += 1
            tok = (dma, self.dma_cnt[dma])
        else:
            tok = (eng, idx)
        for b in reads:
            k, v = tok
            if b.r.get(k, 0) < v:
                b.r[k] = v
        for b in writes:
            b.w = tok
            b.r = {}
        return tok

    def emit(self):
        nc = self.nc
        sem = {k: self.es.enter_context(nc.semaphore("s_" + k)) for k in self.ENG}
        for k in self.dma_keys:
            sem[k] = self.es.enter_context(nc.semaphore("d_" + k))
        val = {}
        for e in self.ENG:
            c = 0
            arr = []
            for s in self.sig[e]:
                if s:
                    c += 1
                arr.append(c)
            val[e] = arr
        n_ins = 0
        for e in self.ENG:
            h = self.h[e]
            waited = {}
            for i, (fn, deps, dma) in enumerate(self.ops[e]):
                for k, v in deps.items():
                    if k in val:
                        sv = val[k][v - 1]
                    else:
                        sv = 16 * v
                    if waited.get(k, 0) >= sv:
                        continue
                    h.wait_ge(sem[k], sv)
                    waited[k] = sv
                ins = fn(h)
                n_ins += 1
                if dma is not None:
                    ins.then_inc(sem[dma], 16)
                elif self.sig[e][i]:
                    ins.then_inc(sem[e], 1)
        self.sem = sem
        return n_ins


def _unit(W, cols, k0, k1):
    sub = W[k0 * 128:k1 * 128][:, cols]
    sub = sub.reshape(k1 - k0, 128, sub.shape[1]).transpose(1, 0, 2)
    return np.ascontiguousarray(sub).reshape(128, -1)


def _vec(v):
    return np.ascontiguousarray(v.reshape(-1, 128).T)


def w_in_unit_cols(l):
    units = []
    units.append(("lo0", np.arange(2560, 2688)))
    units.append(("lo1", np.arange(2688, 2816)))
    if l > 0:
        units.append(("lo2", np.arange(2816, 2848)))
    for j in range(4):
        units.append((f"r{j}", np.arange(1024 + j * 128, 1024 + (j + 1) * 128)))
        units.append((f"k{j}", np.arange(1536 + j * 128, 1536 + (j + 1) * 128)))
        units.append((f"v{j}", np.arange(2048 + j * 128, 2048 + (j + 1) * 128)))
    for i in range(4):
        units.append((f"ca{i}", np.arange(i * 128, (i + 1) * 128)))
        units.append((f"cg{i}", np.arange(512 + i * 128, 512 + (i + 1) * 128)))
    return units


def layer_units(l):
    out = []
    for name, cols in w_in_unit_cols(l):
        out.append(("in_" + name, 8 * len(cols)))
    for n in range(8):
        out.append((f"out{n}", 1024))
    for n in range(8):
        out.append((f"q{n}", 1024))
    for n in range(8):
        out.append((f"o{n}", 1024))
    for i in range(NFF):
        out.append((f"g{i}", 1024))
        out.append((f"u{i}", 1024))
    for n in range(8):
        out.append((f"d{n}a", 1024))
        out.append((f"d{n}b", 1024))
        out.append((f"d{n}c", 6 * 128))
    return out


def prep_layer_weights(l, inp):
    w_in = inp["w_in_first"] if l == 0 else inp["w_in_rest"][l - 1]
    parts = []
    for name, cols in w_in_unit_cols(l):
        parts.append(_unit(w_in, cols, 0, 8))
    for W in (inp["w_out"][l], inp["wq"][l], inp["wo"][l]):
        for n in range(8):
            parts.append(_unit(W, np.arange(n * 128, (n + 1) * 128), 0, 8))
    wgu = inp["w_gu"][l]
    for i in range(NFF):
        parts.append(_unit(wgu, np.arange(i * 128, (i + 1) * 128), 0, 8))
        parts.append(_unit(wgu, np.arange(DFF + i * 128, DFF + (i + 1) * 128), 0, 8))
    wd = inp["w_down"][l]
    for n in range(8):
        cols = np.arange(n * 128, (n + 1) * 128)
        parts.append(_unit(wd, cols, 0, 8))
        parts.append(_unit(wd, cols, 8, 16))
        parts.append(_unit(wd, cols, 16, 22))
    return np.ascontiguousarray(np.concatenate(parts, axis=1))


PC = {}
_o = 0
for _name, _n in (("g", 48), ("mu", 15), ("cw", 124), ("cb", 4), ("lng", 4), ("lnb", 4), ("w0", 4), ("a0", 4),
                  ("v0", 4), ("kk", 4), ("ka", 4), ("rk", 4), ("xg", 4), ("xb", 4)):
    PC[_name] = _o
    _o += _n
NPAR = _o


def prep_params(l, inp):
    P = np.zeros((128, NPAR), np.float32)
    g = inp["norm_gains"][l]
    for i in range(6):
        P[:, PC["g"] + i * 8: PC["g"] + (i + 1) * 8] = _vec(g[i])
    mu = inp["mu_first"] if l == 0 else inp["mu_rest"][l - 1]
    mu_p = np.zeros(15 * 128, np.float32)
    mu_p[:mu.shape[0]] = mu
    P[:, PC["mu"]:PC["mu"] + 15] = _vec(mu_p)
    cw = inp["conv_w"][l]
    P[:, PC["cw"]:PC["cw"] + 124] = np.ascontiguousarray(
        cw.reshape(CW, 4, 128).transpose(2, 1, 0)).reshape(128, 124)
    for name, key in (("cb", "conv_b"), ("lng", "conv_ln_g"), ("lnb", "conv_ln_b"), ("w0", "w0"), ("a0", "a0"),
                      ("kk", "k_k"), ("ka", "k_a"), ("xg", "lnx_g"), ("xb", "lnx_b")):
        P[:, PC[name]:PC[name] + 4] = _vec(inp[key][l])
    P[:, PC["rk"]:PC["rk"] + 4] = _vec(inp["r_k"][l].reshape(-1))
    if l > 0:
        P[:, PC["v0"]:PC["v0"] + 4] = _vec(inp["v0"][l - 1])
    return P


def prep_lora(l, inp):
    A = np.zeros((128, 3, 512), np.float32)
    A[0:64, 0] = inp["w_up"][l]
    A[64:128, 0] = inp["a_up"][l]
    A[:, 1] = inp["g_up"][l]
    if l > 0:
        A[0:32, 2] = inp["v_up"][l - 1]
    return A


CONST_W = 1412


def make_consts():
    C = np.zeros((128, CONST_W), np.float32)
    p = np.arange(128)
    h = p // 64
    j = p % 64
    same = (h[:, None] == h[None, :])
    C[:, 0:128] = np.eye(128)
    mus = (same & (j[:, None] < j[None, :])).astype(np.float32)
    C[:, 128:256] = mus
    C[:, 256:384] = -mus
    mls = (same & (j[None, :] < j[:, None])).astype(np.float32)
    C[:, 384:512] = -mls
    t = np.arange(64)
    mui = (j[:, None] <= t[None, :]).astype(np.float32)
    C[:, 512:576] = mui
    C[:, 576:640] = -mui
    C[:, 640:768] = same.astype(np.float32) / 64.0
    C[:, 768:896] = same.astype(np.float32)
    C[:, 896:1024] = 1.0 / 1024.0
    C[:, 1024:1152] = 1.0 / 512.0
    C[:, 1152:1280] = 1.0
    C[:, 1280:1408] = -np.eye(128)
    C[:, 1408] = 1e-6
    C[:, 1409] = 1e-5
    C[:, 1410] = 64e-5
    C[:, 1411] = 1e-24
    return C


def build(NT=SEQ // 256, NL=DEPTH, TT=256):
    NCH = TT // 64
    nc = bass.Bass("TRN2", target_bir_lowering=False)
    es = ExitStack()
    S = Sched(nc, es)

    xT = nc.dram_tensor("xT", [D, NT * TT], F32, kind="ExternalInput").ap()
    memT = nc.dram_tensor("memT", [D, MEM], F32, kind="ExternalInput").ap()
    memg = nc.dram_tensor("memg", [128, 8], F32, kind="ExternalInput").ap()
    consts_d = nc.dram_tensor("consts", [128, CONST_W], F32, kind="ExternalInput").ap()
    par_d = nc.dram_tensor("par", [NL, 128, NPAR], F32, kind="ExternalInput").ap()
    lora_d = nc.dram_tensor("lora", [NL, 128, 3 * 512], F32, kind="ExternalInput").ap()
    wkv_d = nc.dram_tensor("wkvu", [NL, 128, 16 * 1024], F32, kind="ExternalInput").ap()
    lunits = [layer_units(l) for l in range(NL)]
    wl_d = [nc.dram_tensor(f"wl{l}", [128, sum(n for _, n in lunits[l])], F32, kind="ExternalInput").ap()
            for l in range(NL)]
    outT = nc.dram_tensor("outT", [D, NT * TT], F32, kind="ExternalOutput").ap()
    wlb_d = [nc.dram_tensor(f"wlb{l}", [128, sum(n for _, n in lunits[l])], BF16).ap() for l in range(NL)]
    if DEBUG:
        dbg_d = nc.dram_tensor("dbg", [128, KC * TT], F32, kind="ExternalOutput").ap()

    def sb(name, shape, dt):
        return es.enter_context(nc.sbuf_tensor(name, shape, dt))

    def ps_t(name, shape, dt):
        return es.enter_context(nc.psum_tensor(name, shape, dt))

    X = sb("X", [128, KC, TT], F32)
    Xc = [T(X[:, c, :]) for c in range(KC)]
    Hh = sb("H", [128, KC, TT], BF16)
    Hc = [T(Hh[:, c, :]) for c in range(KC)]
    MIXb = sb("MIX", [128, KC, TT], BF16)
    MIXc = [T(MIXb[:, c, :]) for c in range(KC)]
    Y = sb("Y", [128, KC, TT], F32)
    Yc = [T(Y[:, c, :]) for c in range(KC)]
    KT = [sb(f"KT{l}", [128, KC, MEM], BF16) for l in range(NL)]
    KTt = [T(KT[l][:]) for l in range(NL)]
    VM = [sb(f"VM{l}", [128, 2, D], BF16) for l in range(NL)]
    VMt = [T(VM[l][:]) for l in range(NL)]
    RING = sb("ring", [128, NSLOT, 1024], BF16)
    ring = [T(RING[:, s, :]) for s in range(NSLOT)]
    VF = sb("VF", [128, 4, TT], F32)
    VFc = [T(VF[:, j, :]) for j in range(4)]
    HS = [[T(sb(f"HS{l}_{j}", [128, 128], F32)[:]) for j in range(4)] for l in range(NL)]
    HSb = [[[T(sb(f"HSb{l}_{j}_{k}", [128, 128], BF16)[:]) for k in range(2)] for j in range(4)] for l in range(NL)]
    hsb_par = [[0] * 4 for _ in range(NL)]
    PAR = [T(sb(f"PAR{l}", [128, NPAR], F32)[:]) for l in range(NL)]
    LORA = [T(sb(f"LORA{l}", [128, 3, 512], BF16)[:]) for l in range(NL)]
    CH = [T(sb(f"CH{l}", [128, 4, CW - 1], F32)[:]) for l in range(NL)]
    SH = [T(sb(f"SH{l}", [128, 15], F32)[:]) for l in range(NL)]
    CF = T(sb("CF", [128, CONST_W], F32)[:])
    CB = T(sb("CB", [128, CONST_W], BF16)[:])

    def cf(a, b):
        return CF[:, a:b]

    def cb(a, b):
        return CB[:, a:b]
    IDb = cb(0, 128)
    MUS, NMUS, NMLS = cf(128, 256), cf(256, 384), cf(384, 512)
    MUI, NMUI = cf(512, 576), cf(576, 640)
    IDf = cf(0, 128)
    BO64b, BO1b = cb(640, 768), cb(768, 896)
    O1024b, O512b, O1b = cb(896, 1024), cb(1024, 1152), cb(1152, 1280)

    RSTD = T(sb("RSTD", [128, TT], F32)[:])
    SQ = [T(sb(f"SQ{i}", [128, TT], BF16)[:]) for i in range(2)]
    sq_i = [0]
    PT = [T(sb(f"PT{i}", [128, TT + 1], F32)[:]) for i in range(3)]
    pt_i = [0]
    TMP = [T(sb(f"TMP{i}", [128, TT], F32)[:]) for i in range(4)]
    tmp_i = [0]

    def tmp():
        t = TMP[tmp_i[0] % len(TMP)]
        tmp_i[0] += 1
        return t
    LOb = T(sb("LOb", [128, TT], BF16)[:])
    Gb = T(sb("Gb", [128, TT], BF16)[:])
    VRb = T(sb("VRb", [32, TT], BF16)[:])
    RAW = [[T(sb(f"RAW{i}_{q}", [128, TT], F32)[:]) for q in range(3)] for i in range(2)]
    RAW.append([Yc[0], Yc[1], Yc[2]])
    RAW.append([Yc[3], Yc[4], Yc[5]])
    P0 = sb("P0", [128, NCH, 96], F32)
    P1 = sb("P1", [128, NCH, 96], F32)
    P0t, P1t = T(P0[:]), T(P1[:])
    At = T(sb("A_t", [128, TT], F32)[:])
    KKt = T(sb("KK_t", [128, TT], F32)[:])
    Kpt = T(sb("Kp_t", [128, TT], F32)[:])
    Bvt = T(sb("Bv_t", [128, TT], F32)[:])
    EX = [T(sb(f"EX{i}", [128, NCH, 64], F32)[:]) for i in range(4)]
    GCt = [T(sb(f"GC{i}", [128, NCH], F32)[:]) for i in range(2)]
    NSET = 2
    EXPN = ("KKe", "KTe", "BTe", "KHe", "BHe", "Ve")
    EXP = [{n: T(sb(f"{n}{i}", [128, NCH, 128], BF16)[:]) for n in EXPN} for i in range(NSET)]
    RT = [T(sb(f"Rt{i}", [128, TT], BF16)[:]) for i in range(NSET)]
    Gt = [T(sb(f"G_t{i}", [128, TT], F32)[:]) for i in range(NSET)]
    BON = [T(sb(f"BON{i}", [128, TT], F32)[:]) for i in range(NSET)]
    YR = [T(sb(f"YR{i}", [128, TT], F32)[:]) for i in range(NSET)]
    MATN = ("LkkT", "M1", "N1", "Sa", "Sb", "Ma", "Mb", "Na", "Nb", "Vbd", "KHbd", "BHbdn", "KKbd", "X1", "TKT", "Usb")
    NMSET = NCH
    MATS = []
    for i in range(min(2, NMSET)):
        d_ = {n: T(sb(f"m_{n}{i}", [128, 128], BF16)[:]) for n in MATN}
        d_["MrkT"] = T(sb(f"m_MrkT{i}", [128, 64], BF16)[:])
        d_["MrbTn"] = T(sb(f"m_MrbTn{i}", [128, 64], BF16)[:])
        MATS.append(d_)
    mset_i = [0]
    HG = sb("HG", [128, 4, CW - 1 + TT], F32)
    HGc = [T(HG[:, i, :]) for i in range(4)]
    ACC = sb("ACC", [128, 4, TT], F32)
    ACCc = [T(ACC[:, i, :]) for i in range(4)]
    MEANt = T(sb("MEANt", [128, TT], F32)[:])
    VARt = T(sb("VARt", [128, TT], F32)[:])
    Q = sb("Q", [128, KC, TT], BF16)
    Qc = [T(Q[:, c, :]) for c in range(KC)]
    E = [T(sb(f"E{i}", [128, 2, TT], BF16)[:]) for i in range(2)]
    RD = [T(sb(f"RD{i}", [128, TT], F32)[:]) for i in range(2)]
    Oo = sb("Oo", [128, KC, TT], BF16)
    Oc = [T(Oo[:, c, :]) for c in range(KC)]
    AV = sb("AV", [128, NFF, TT], BF16)
    AVc = [T(AV[:, i, :], [Buf() for _ in range(TT // 128)]) for i in range(NFF)]
    _slots = [(i, k) for i in range(NFF) for k in range(TT // 128)]
    _si = 0
    for i in range(2, NMSET):
        d_ = {}
        for n in list(MATN) + ["MrkT", "MrbTn"]:
            ci, k = _slots[_si]
            _si += 1
            wdt = 64 if n in ("MrkT", "MrbTn") else 128
            d_[n] = T(AV[:, ci, k * 128:k * 128 + wdt], [AVc[ci].bufs[k]])
        MATS.append(d_)
    MN = T(Hh[:, :, 0:MEM], [b for t in Hc for b in t.bufs])
    MF = T(Y[:, :, 0:MEM], [b for t in Yc for b in t.bufs])
    MG = T(sb("MG", [128, 8], F32)[:])
    VTt = T(sb("VTt", [128, MEM], BF16)[:])

    PSB = [ps_t(f"psb{i}", [128, 512], F32) for i in range(8)]
    PSBT = [T(PSB[b][:]) for b in range(8)]
    pb_i = [0]

    def pbig():
        t = PSBT[pb_i[0] % 8]
        pb_i[0] += 1
        return t[:, 0:TT]

    def psm():
        t = PSBT[pb_i[0] % 8]
        pb_i[0] += 1
        return t[:, 0:128]

    def bufs_of(*ts):
        out = []
        for t in ts:
            if isinstance(t, T):
                out.extend(t.bufs)
        return out

    def apof(x):
        return x.ap if isinstance(x, T) else x

    def MM(out, lhsT, rhs, start=True, stop=True):
        S.op("pe", lambda e: e.matmul(out.ap, lhsT=lhsT.ap, rhs=rhs.ap, start=start, stop=stop),
             bufs_of(lhsT, rhs), out.bufs)

    def ACT(out, in_, func, bias=None, scale=1.0):
        kw = {}
        if bias is not None:
            kw["bias"] = apof(bias)
        S.op("act", lambda e: e.activation(out=out.ap, in_=in_.ap, func=func, scale=apof(scale), **kw),
             bufs_of(in_, bias, scale), out.bufs)

    def TTo(out, in0, in1, op, eng="dve"):
        S.op(eng, lambda e: e.tensor_tensor(out=out.ap, in0=in0.ap, in1=in1.ap, op=op),
             bufs_of(in0, in1), out.bufs)

    def TS(out, in0, s1, op0, s2=None, op1=None, eng="dve"):
        if op1 is None:
            S.op(eng, lambda e: e.tensor_scalar(out=out.ap, in0=in0.ap, scalar1=apof(s1), scalar2=None, op0=op0),
                 bufs_of(in0, s1), out.bufs)
        else:
            S.op(eng, lambda e: e.tensor_scalar(out=out.ap, in0=in0.ap, scalar1=apof(s1), scalar2=apof(s2),
                                                op0=op0, op1=op1),
                 bufs_of(in0, s1, s2), out.bufs)

    def STT(out, in0, sc, in1, op0, op1, eng="dve"):
        S.op(eng, lambda e: e.scalar_tensor_tensor(out=out.ap, in0=in0.ap, scalar=apof(sc), in1=in1.ap,
                                                   op0=op0, op1=op1),
             bufs_of(in0, sc, in1), out.bufs)

    def CP(out, in_, eng="dve"):
        if eng == "act":
            ACT(out, in_, AF.Copy)
        else:
            S.op(eng, lambda e: e.tensor_copy(out=out.ap, in_=in_.ap), in_.bufs, out.bufs)

    def MEMSET(t, val, eng="dve"):
        S.op(eng, lambda e: e.memset(t.ap, val), (), t.bufs)

    def DMA(eng, out, in_ap, key):
        S.op(eng, lambda e: e.dma_start(out=out.ap, in_=in_ap), (), out.bufs, dma=key)

    def DMAOUT(eng, out_ap, in_, key):
        S.op(eng, lambda e: e.dma_start(out=out_ap, in_=in_.ap), in_.bufs, (), dma=key)

    EPSC = {1e-6: 1408, 1e-5: 1409, 64e-5: 1410, 1e-24: 1411}

    def RSQRT(out, in_, eps):
        c = EPSC[eps]
        ACT(out, in_, AF.Sqrt, bias=CF[:, c:c + 1])
        S.op("dve", lambda e: e.reciprocal(out=out.ap, in_=out.ap), out.bufs, out.bufs)

    evac_i = [0]

    def EVAC(out, in_):
        evac_i[0] += 1
        if evac_i[0] % 4:
            CP(out, in_, "act")
        else:
            CP(out, in_, "dve")

    for s in range(NSLOT):
        S.new_dma_sem(f"w{s}")
    wseq = []
    ubufs = {}
    for s_ in range(NSLOT):
        S.new_dma_sem(f"ws{s_}")
    for l in range(NL):
        for n in range(16):
            wseq.append((wkv_d[l, :, n * 1024:(n + 1) * 1024], 1024, f"kv{l}_{n}", None))
    for t in range(NT):
        for l in range(NL):
            off = 0
            for name, ncols in lunits[l]:
                ub = ubufs.setdefault((l, name), Buf())
                wseq.append((wl_d[l][:, off:off + ncols], ncols, f"L{l}_{name}",
                             (wlb_d[l][:, off:off + ncols], ub, t)))
                off += ncols
    wst = {"issue": 0, "use": 0}

    def wget(expect):
        while wst["issue"] < min(len(wseq), wst["use"] + NSLOT):
            u = wst["issue"]
            ap, ncols, _, binfo = wseq[u]
            slot = u % NSLOT
            dst = ring[slot][:, 0:ncols]
            if binfo is None or (binfo[2] == 0 and NT == 1):
                DMA("pool", dst, ap, f"w{slot}")
            elif binfo[2] == 0:
                DMA("pool", dst, ap, f"w{slot}")
                bap, ub, _t = binfo
                S.op("sp", lambda e, o=bap, i=dst.ap: e.dma_start(out=o, in_=i), dst.bufs, [ub], dma=f"ws{slot}")
            else:
                bap, ub, _t = binfo
                S.op("sp", lambda e, o=dst.ap, i=bap: e.dma_start(out=o, in_=i), [ub], dst.bufs, dma=f"w{slot}")
            wst["issue"] += 1
        u = wst["use"]
        assert wseq[u][2] == expect, (wseq[u][2], expect)
        wst["use"] += 1
        return ring[u % NSLOT]

    def setup_dma(eng, out, in_ap, key):
        S.new_dma_sem(key)
        DMA(eng, out, in_ap, key)
    for k in ("xin", "xout"):
        S.new_dma_sem(k)
    setup_dma("sp", CF, consts_d[:, :], "cst")
    setup_dma("pool", CB, consts_d[:, :], "cstb")
    for l in range(NL):
        setup_dma("sp", PAR[l], par_d[l], f"par{l}")
    for l in range(NL):
        setup_dma("pool", LORA[l], lora_d[l].rearrange("p (a b) -> p a b", a=3), f"lora{l}")
    setup_dma("sp", MF, memT.rearrange("(c p) m -> p c m", p=128), "mem")
    setup_dma("sp", MG, memg[:, :], "memg")
    for l in range(NL):
        for j in range(4):
            MEMSET(HS[l][j], 0.0)
            MEMSET(HSb[l][j][0], 0.0)
            MEMSET(HSb[l][j][1], 0.0)
        MEMSET(CH[l], 0.0)
        MEMSET(SH[l], 0.0)
    for i in range(NSET):
        for n in EXPN:
            MEMSET(EXP[i][n], 0.0)
    MEMSET(P0t, 0.0)
    MEMSET(P1t, 0.0)

    def sqtile():
        t = SQ[sq_i[0] % 2]
        sq_i[0] += 1
        return t

    def rms_stats(src_list, out_rstd, ncol, ones_b, eps):
        p = pbig()
        n = len(src_list)
        for i, s in enumerate(src_list):
            q = sqtile()
            ACT(q[:, 0:ncol], s, AF.Square)
            MM(p[:, 0:ncol], ones_b, q[:, 0:ncol], start=(i == 0), stop=(i == n - 1))
        RSQRT(out_rstd, p[:, 0:ncol], eps)

    MFc = [MF[:, c, :] for c in range(KC)]
    MRS = T(sb("MRS", [128, MEM], F32)[:])
    rms_stats(MFc, MRS, MEM, O1024b, 1e-6)
    for c in range(KC):
        STT(MN[:, c, :], MF[:, c, :], MG[:, c:c + 1], MRS, ALU.mult, ALU.mult)
    for l in range(NL):
        for n in range(16):
            w = wget(f"kv{l}_{n}")
            p = pbig()
            for kc in range(KC):
                MM(p[:, 0:MEM], w[:, kc * 128:(kc + 1) * 128], MN[:, kc, :], start=(kc == 0), stop=(kc == KC - 1))
            if n < 8:
                EVAC(KTt[l][:, n, :], p[:, 0:MEM])
            else:
                EVAC(VTt, p[:, 0:MEM])
                for mc in range(2):
                    q = psm()
                    MM(q, VTt[:, mc * 128:(mc + 1) * 128], IDb)
                    EVAC(VMt[l][:, mc, (n - 8) * 128:(n - 7) * 128], q)

    def pre_norm(l, gi):
        rms_stats(Xc, RSTD, TT, O1024b, 1e-6)
        for c in range(KC):
            col = PC["g"] + gi * 8 + c
            STT(Hc[c], Xc[c], PAR[l][:, col:col + 1], RSTD, ALU.mult, ALU.mult)

    def post_norm(l, gi):
        rms_stats(Yc, RSTD, TT, O1024b, 1e-6)
        for c in range(KC):
            col = PC["g"] + gi * 8 + c
            t = tmp()
            STT(t, Yc[c], PAR[l][:, col:col + 1], RSTD, ALU.mult, ALU.mult)
            TTo(Xc[c], Xc[c], t, ALU.add)

    def linear8(l, prefix, src, nout, consume):
        for n in range(nout):
            w = wget(f"L{l}_{prefix}{n}")
            p = pbig()
            for kc in range(KC):
                MM(p, w[:, kc * 128:(kc + 1) * 128], src[kc], start=(kc == 0), stop=(kc == KC - 1))
            consume(n, p)

    def rwkv_pre(l, j, raw, si):
        R, K, V = raw
        P = PAR[l]
        ex = EXP[si]
        pc = lambda name: P[:, PC[name] + j:PC[name] + j + 1]
        lora = LORA[l]
        jc = slice(j * 128, (j + 1) * 128)
        v3 = lambda t: t.v(t.ap.rearrange("p (c t) -> p c t", t=64))
        p = pbig()
        MM(p, lora[0:64, 0, jc], LOb[0:64, :])
        t1 = tmp()
        ACT(t1, p, AF.Sigmoid, bias=pc("w0"))
        LG = P0t[:, :, 32:96]
        TS(LG, v3(t1), -DECAY_SCALE, ALU.mult)
        yield
        p = pbig()
        MM(p, lora[64:128, 0, jc], LOb[64:128, :])
        ACT(At, p, AF.Sigmoid, bias=pc("a0"))
        p = pbig()
        MM(p, lora[:, 1, jc], Gb)
        CP(Gt[si], p, "act")
        yield
        if l == 0:
            CP(VFc[j], V, "dve")
        else:
            p = pbig()
            MM(p, lora[0:32, 2, jc], VRb[0:32, :])
            t2 = tmp()
            ACT(t2, p, AF.Sigmoid, bias=pc("v0"))
            t3 = tmp()
            TTo(t3, VFc[j], V, ALU.subtract)
            TTo(t3, t3, t2, ALU.mult)
            TTo(V, V, t3, ALU.add)
        yield
        TS(KKt, K, pc("kk"), ALU.mult)
        q = sqtile()
        ACT(q, KKt, AF.Square)
        p = pbig()
        MM(p, BO1b, q)
        t4 = tmp()
        RSQRT(t4, p, 1e-24)
        TTo(KKt, KKt, t4, ALU.mult)
        yield
        t5 = tmp()
        TS(t5, At, pc("ka"), ALU.mult, pc("ka"), ALU.subtract)
        STT(Kpt, t5, 1.0, K, ALU.add, ALU.mult)
        TTo(Bvt, KKt, At, ALU.mult)
        yield
        q = sqtile()
        STT(q, R, pc("rk"), Kpt, ALU.mult, ALU.mult)
        p = pbig()
        MM(p, BO1b, q)
        TTo(BON[si], p, V, ALU.mult)
        yield
        src, dst = P0t, P1t
        for s_ in (1, 2, 4, 8, 16, 32):
            TTo(dst[:, :, 32:96], src[:, :, 32:96], src[:, :, 32 - s_:96 - s_], ALU.add)
            src, dst = dst, src
            yield
        assert src is P0t
        LGP = P0t[:, :, 31:95]
        ACT(EX[0], LG, AF.Exp)
        ACT(EX[1], LGP, AF.Exp)
        ACT(EX[2], LG, AF.Exp, scale=-1.0)
        gc = GCt[si]
        ACT(gc, P0t[:, :, 95], AF.Exp)
        yield
        TTo(EX[3], EX[2], gc.v(gc.ap.unsqueeze(2).to_broadcast([128, NCH, 64])), ALU.mult)
        TTo(RT[si], R, EX[0].v(EX[0].ap.rearrange("p c t -> p (c t)")), ALU.mult)
        yield
        for h in range(2):
            ph = slice(h * 64, (h + 1) * 64)
            fh = slice(h * 64, (h + 1) * 64)
            TTo(ex["KKe"][ph, :, fh], v3(KKt)[ph], EX[1][ph], ALU.mult)
            TTo(ex["KTe"][ph, :, fh], v3(Kpt)[ph], EX[2][ph], ALU.mult)
            yield
            TTo(ex["BTe"][ph, :, fh], v3(Bvt)[ph], EX[2][ph], ALU.mult)
            TTo(ex["KHe"][ph, :, fh], v3(Kpt)[ph], EX[3][ph], ALU.mult)
            yield
            TTo(ex["BHe"][ph, :, fh], v3(Bvt)[ph], EX[3][ph], ALU.mult)
            CP(ex["Ve"][ph, :, fh], v3(V)[ph], "dve")
            yield

    def rwkv_chunks(l, j, si):
        ex = EXP[si]
        ms = [MATS[c % NMSET] for c in range(NCH)]
        assert NMSET >= NCH
        opnd = []
        for c in range(NCH):
            opnd.append([ex[n][:, c, :] for n in EXPN] + [RT[si][:, c * 64:(c + 1) * 64]])
        for c in range(NCH):
            m = ms[c]
            KKe, KTe, BTe, KHe, BHe, Ve, Rc = opnd[c]
            p = psm(); MM(p, KTe, KKe); TTo(m["LkkT"], p, MUS, ALU.mult)
            p = psm(); MM(p, BTe, KKe); TTo(m["M1"], p, NMUS, ALU.mult)
            p = psm(); MM(p, KKe, BTe); TTo(m["N1"], p, NMLS, ALU.mult)
            yield
            p = psm(); MM(p[:, 0:64], KTe, Rc); TTo(m["MrkT"], p[:, 0:64], MUI, ALU.mult)
            p = psm(); MM(p[:, 0:64], BTe, Rc); TTo(m["MrbTn"], p[:, 0:64], NMUI, ALU.mult)
            TTo(m["Sa"], m["M1"], IDf, ALU.add)
            yield
            p = psm(); MM(p, Ve, IDb); EVAC(m["Vbd"], p)
            p = psm(); MM(p, KHe, IDb); EVAC(m["KHbd"], p)
            yield
            p = psm(); MM(p, BHe, cb(1280, 1408)); EVAC(m["BHbdn"], p)
            p = psm(); MM(p, KKe, IDb); EVAC(m["KKbd"], p)
            yield
        cur = [[ms[c]["M1"], ms[c]["N1"], ms[c]["Sa"], ms[c]["Ma"], ms[c]["Na"], ms[c]["Sb"]] for c in range(NCH)]
        for lvl in range(5):
            last = (lvl == 4)
            for c in range(NCH):
                m = ms[c]
                Mc, Nc, Sc, Mn, Nn, Sn = cur[c]
                if not last:
                    p = psm(); MM(p, Nc, Mc); EVAC(Mn, p)
                p = psm(); MM(p, Mc, Nc); EVAC(Nn, p)
                yield
            for c in range(NCH):
                m = ms[c]
                Mc, Nc, Sc, Mn, Nn, Sn = cur[c]
                p = psm()
                MM(p, Nn, Sc, start=True, stop=False)
                MM(p, IDb, Sc, start=False, stop=True)
                EVAC(Sn, p)
                if lvl == 0:
                    cur[c] = [Mn, Nn, Sn, m["Mb"], m["Nb"], m["Sa"]]
                else:
                    cur[c] = [Mn, Nn, Sn, Mc, Nc, Sc]
                yield
        for c in range(NCH):
            m = ms[c]
            TT_ = cur[c][2]
            p = psm(); MM(p, m["LkkT"], m["Vbd"]); EVAC(m["X1"], p)
            p = psm(); MM(p, m["KKbd"], TT_); EVAC(m["TKT"], p)
            yield
        hs = HS[l][j]
        for c in range(NCH):
            m = ms[c]
            TT_ = cur[c][2]
            Rc = opnd[c][6]
            gcol = GCt[si][:, c:c + 1]
            hb_old = HSb[l][j][hsb_par[l][j]]
            hb_new = HSb[l][j][1 - hsb_par[l][j]]
            hsb_par[l][j] = 1 - hsb_par[l][j]
            p = psm()
            MM(p, TT_, m["X1"], start=True, stop=False)
            MM(p, m["TKT"], hb_old, start=False, stop=True)
            CP(m["Usb"], p, "act")
            p = psm()
            MM(p, m["KHbd"], m["Vbd"], start=True, stop=False)
            MM(p, m["BHbdn"], m["Usb"], start=False, stop=True)
            STT(hs, hs, gcol, p, ALU.mult, ALU.add)
            CP(hb_new, hs, "act")
            py = psm()
            MM(py[:, 0:64], hb_old, Rc, start=True, stop=False)
            MM(py[:, 0:64], m["Vbd"], m["MrkT"], start=False, stop=False)
            MM(py[:, 0:64], m["Usb"], m["MrbTn"], start=False, stop=True)
            CP(YR[si][:, c * 64:(c + 1) * 64], py[:, 0:64], "act")
            yield

    def rwkv_post(l, j, si):
        P = PAR[l]
        pc = lambda name: P[:, PC[name] + j:PC[name] + j + 1]
        yr = YR[si]
        q = sqtile()
        CP(q, yr, "act")
        pm = pbig()
        MM(pm, BO64b, q)
        q2 = sqtile()
        ACT(q2, yr, AF.Square)
        pe2 = pbig()
        MM(pe2, BO64b, q2)
        mean = tmp()
        CP(mean, pm, "act")
        m2 = tmp()
        TTo(m2, mean, mean, ALU.mult)
        var = tmp()
        TTo(var, pe2, m2, ALU.subtract)
        RSQRT(var, var, 64e-5)
        t1 = tmp()
        TTo(t1, yr, mean, ALU.subtract)
        TTo(t1, t1, var, ALU.mult)
        TS(t1, t1, pc("xg"), ALU.mult, pc("xb"), ALU.add)
        TTo(t1, t1, BON[si], ALU.add)
        TTo(MIXc[4 + j], t1, Gt[si], ALU.mult)

    def conv_task(l):
        P = PAR[l]
        for jj in range(CW):
            for i in range(4):
                wcol = P[:, PC["cw"] + i * CW + jj:PC["cw"] + i * CW + jj + 1]
                if jj == 0:
                    TS(ACCc[i], HGc[i][:, 0:TT], wcol, ALU.mult, P[:, PC["cb"] + i:PC["cb"] + i + 1], ALU.add,
                       eng=CONV_ENG)
                else:
                    STT(ACCc[i], HGc[i][:, jj:jj + TT], wcol, ACCc[i], ALU.mult, ALU.add, eng=CONV_ENG)
            yield
        pm = pbig()
        pe2 = pbig()
        for i in range(4):
            q = sqtile()
            CP(q, ACCc[i], "act")
            MM(pm, O512b, q, start=(i == 0), stop=(i == 3))
            q2 = sqtile()
            ACT(q2, ACCc[i], AF.Square)
            MM(pe2, O512b, q2, start=(i == 0), stop=(i == 3))
        CP(MEANt, pm, "act")
        m2 = tmp()
        TTo(m2, MEANt, MEANt, ALU.mult)
        var = VARt
        TTo(var, pe2, m2, ALU.subtract)
        RSQRT(var, var, 1e-5)
        yield
        for i in range(4):
            t1 = tmp()
            TTo(t1, ACCc[i], MEANt, ALU.subtract)
            TTo(t1, t1, var, ALU.mult)
            ACT(MIXc[i], t1, AF.Silu, bias=P[:, PC["lnb"] + i:PC["lnb"] + i + 1],
                scale=P[:, PC["lng"] + i:PC["lng"] + i + 1])
            yield

    def mixer(l):
        P = PAR[l]
        pre_norm(l, 0)
        for i in range(4):
            CP(HGc[i][:, 0:CW - 1], CH[l][:, i, :], "act")
        names = [n for n, _ in w_in_unit_cols(l)]
        state = {}
        for name in names:
            w = wget(f"L{l}_in_{name}")
            ncol = 32 if name == "lo2" else 128
            p = pbig()
            for kc in range(KC):
                MM(p[0:ncol, :], w[:, kc * ncol:(kc + 1) * ncol], Hc[kc], start=(kc == 0), stop=(kc == KC - 1))
            kind = name[0]
            if kind in ("l", "r", "k", "v"):
                if kind == "l":
                    midx = 12 + int(name[2])
                else:
                    midx = {"r": 0, "k": 4, "v": 8}[kind] + int(name[1])
                pt = PT[pt_i[0] % len(PT)]
                pt_i[0] += 1
                rows = slice(0, ncol)
                CP(pt[rows, 0:1], SH[l][rows, midx:midx + 1], "act")
                CP(pt[rows, 1:TT + 1], p[rows, :], "act")
                d = tmp()
                TTo(d[rows], pt[rows, 0:TT], pt[rows, 1:TT + 1], ALU.subtract)
                CP(SH[l][rows, midx:midx + 1], pt[rows, TT:TT + 1], "act")
                mucol = P[rows, PC["mu"] + midx:PC["mu"] + midx + 1]
                if kind == "l":
                    sh = tmp()
                    STT(sh[rows], d[rows], mucol, pt[rows, 1:TT + 1], ALU.mult, ALU.add)
                    if name == "lo0":
                        ACT(LOb[0:64, :], sh[0:64, :], AF.Tanh)
                        CP(LOb[64:128, :], sh[64:128, :], "act")
                    elif name == "lo1":
                        ACT(Gb, sh, AF.Sigmoid)
                    else:
                        CP(VRb[0:32, :], sh[0:32, :], "act")
                else:
                    j = int(name[1])
                    dst = RAW[j][{"r": 0, "k": 1, "v": 2}[kind]]
                    STT(dst, d, mucol, pt[:, 1:TT + 1], ALU.mult, ALU.add)
            else:
                i = int(name[2])
                if name[1] == "a":
                    state["pa"] = p
                else:
                    sg = tmp()
                    ACT(sg, p, AF.Sigmoid)
                    TTo(HGc[i][:, CW - 1:CW - 1 + TT], state["pa"], sg, ALU.mult)
        for i in range(4):
            CP(CH[l][:, i, :], HGc[i][:, TT:TT + CW - 1], "act")
        prog = {"pre": 0, "chunks": 0}

        def thread_pre():
            for j in range(4):
                while prog["chunks"] < j - NSET + 1:
                    yield
                yield from rwkv_pre(l, j, RAW[j], j % NSET)
                prog["pre"] = j + 1
                yield

        def thread_chunks():
            for j in range(4):
                while prog["pre"] < j + 1:
                    yield
                yield from rwkv_chunks(l, j, j % NSET)
                prog["chunks"] = j + 1
                rwkv_post(l, j, j % NSET)
                yield

        active = [thread_pre(), thread_chunks(), conv_task(l)]
        while active:
            for g in list(active):
                try:
                    next(g)
                except StopIteration:
                    active.remove(g)
        if DEBUG and l == NL - 1 and dbg_state["t"] == 0:
            DBGT = T(sb("DBGT", [128, KC, TT], F32)[:])
            for c in range(KC):
                CP(DBGT[:, c, :], MIXc[c], "dve")
            S.new_dma_sem("dbg")
            DMAOUT("sp", dbg_d.rearrange("p (c t) -> p c t", c=KC), DBGT, "dbg")
        linear8(l, "out", MIXc, 8, lambda n, p: CP(Yc[n], p, "act"))
        post_norm(l, 1)

    def xattn(l):
        pre_norm(l, 2)
        linear8(l, "q", Hc, 8, lambda n, p: ACT(Qc[n], p, AF.Copy, scale=1.0 / 16.0))
        for h in range(4):
            e = E[h % 2]
            rd = RD[h % 2]
            for mc in range(2):
                p = pbig()
                for dc in range(2):
                    MM(p, KTt[l][:, 2 * h + dc, mc * 128:(mc + 1) * 128], Qc[2 * h + dc], start=(dc == 0), stop=(dc == 1))
                ACT(e[:, mc, :], p, AF.Exp)
            p = pbig()
            for mc in range(2):
                MM(p, O1b, e[:, mc, :], start=(mc == 0), stop=(mc == 1))
            S.op("dve", lambda en, o=rd.ap, i=p.ap: en.reciprocal(out=o, in_=i), p.bufs, rd.bufs)
            for dc in range(2):
                p = pbig()
                for mc in range(2):
                    MM(p, VMt[l][:, mc, (2 * h + dc) * 128:(2 * h + dc + 1) * 128], e[:, mc, :],
                       start=(mc == 0), stop=(mc == 1))
                TTo(Oc[2 * h + dc], p, rd, ALU.mult)
        linear8(l, "o", Oc, 8, lambda n, p: CP(Yc[n], p, "act"))
        post_norm(l, 3)

    def ffn(l):
        pre_norm(l, 4)
        for i in range(NFF):
            wg = wget(f"L{l}_g{i}")
            pg = pbig()
            for kc in range(KC):
                MM(pg, wg[:, kc * 128:(kc + 1) * 128], Hc[kc], start=(kc == 0), stop=(kc == KC - 1))
            wu = wget(f"L{l}_u{i}")
            pu = pbig()
            for kc in range(KC):
                MM(pu, wu[:, kc * 128:(kc + 1) * 128], Hc[kc], start=(kc == 0), stop=(kc == KC - 1))
            sg = tmp()
            ACT(sg, pg, AF.Silu)
            TTo(AVc[i], pu, sg, ALU.mult)
        for n in range(8):
            p = pbig()
            k = 0
            for part, nk in (("a", 8), ("b", 8), ("c", 6)):
                w = wget(f"L{l}_d{n}{part}")
                for kk in range(nk):
                    MM(p, w[:, kk * 128:(kk + 1) * 128], AVc[k], start=(k == 0), stop=(k == NFF - 1))
                    k += 1
            CP(Yc[n], p, "act")
        post_norm(l, 5)

    xT_v = xT.rearrange("(c p) t -> p c t", p=128)
    outT_v = outT.rearrange("(c p) t -> p c t", p=128)
    Xall = T(X[:], [b for t in Xc for b in t.bufs])
    dbg_state = {"t": 0}
    for t in range(NT):
        dbg_state["t"] = t
        DMA("sp", Xall, xT_v[:, :, t * TT:(t + 1) * TT], "xin")
        for l in range(NL):
            last = (l == NL - 1)
            mixer(l)
            if last and STAGE < 2:
                for n in range(8):
                    wget(f"L{l}_q{n}")
                for n in range(8):
                    wget(f"L{l}_o{n}")
            else:
                xattn(l)
            if last and STAGE < 3:
                for i in range(NFF):
                    wget(f"L{l}_g{i}")
                    wget(f"L{l}_u{i}")
                for n in range(8):
                    for part in "abc":
                        wget(f"L{l}_d{n}{part}")
            else:
                ffn(l)
        DMAOUT("sp", outT_v[:, :, t * TT:(t + 1) * TT], Xall, "xout")
    assert wst["use"] == len(wseq), (wst["use"], len(wseq))
    n_ins = S.emit()
    nc.sync.wait_ge(S.sem["xout"], 16 * S.dma_cnt["xout"])
    return nc, es, n_ins


_CACHE = {}


def prep_inputs(inp, NT, NL, TT):
    x = np.asarray(inp["x"], np.float32)
    mem = np.asarray(inp["mem"], np.float32)
    B = x.shape[0]
    Tn = NT * TT
    shared = {
        "memg": _vec(np.asarray(inp["mem_norm_g"], np.float32)),
        "consts": make_consts(),
        "par": np.stack([prep_params(l, inp) for l in range(NL)]),
        "lora": np.stack([prep_lora(l, inp).reshape(128, 1536) for l in range(NL)]),
    }
    wkv = []
    for l in range(NL):
        W = np.asarray(inp["wkv"][l], np.float32)
        wkv.append(np.concatenate([_unit(W, np.arange(n * 128, (n + 1) * 128), 0, 8) for n in range(16)], axis=1))
    shared["wkvu"] = np.ascontiguousarray(np.stack(wkv))
    for l in range(NL):
        shared[f"wl{l}"] = prep_layer_weights(l, inp)
    maps = []
    for b in range(B):
        m = dict(shared)
        m["xT"] = np.ascontiguousarray(x[b, :Tn].T)
        m["memT"] = np.ascontiguousarray(mem[b].T)
        maps.append(m)
    return maps


def run(inp, NT, NL, TT):
    inp = {k: np.asarray(v) for k, v in inp.items()}
    key = (NT, NL, TT)
    nc, es, n_ins = build(NT, NL, TT)
    maps = prep_inputs(inp, NT, NL, TT)
    res = run_bass_kernel_spmd(nc, maps, core_ids=list(range(len(maps))))
    out = np.stack([np.ascontiguousarray(r["outT"].T) for r in res.results])
    if DEBUG:
        global LAST_DBG
        LAST_DBG = np.stack([r["dbg"] for r in res.results])
    return out.astype(np.float32)


def kernel(**inputs):
    return run(inputs, SEQ // 256, DEPTH, 256)
```

```python
import numpy as np
import concourse.bass as bass
import concourse.mybir as mybir
from concourse.bass_utils import run_bass_kernel_spmd
from contextlib import ExitStack

F32 = mybir.dt.float32
BF16 = mybir.dt.bfloat16
AF = mybir.ActivationFunctionType
ALU = mybir.AluOpType

D = 1024
KC = 8
SEQ = 8192
DEPTH = 4
MEM = 256
DFF = 2816
NFF = DFF // 128
CW = 31
DECAY_SCALE = float(np.exp(-0.5))
NSLOT = 6
SAME_ENGINE_WINDOW = 1
CONV_ENG = "dve"
CONV_EVERY = 6
STAGE = 3
DEBUG = False


class Buf:
    __slots__ = ("w", "r")

    def __init__(self):
        self.w = None
        self.r = {}


class T:
    __slots__ = ("ap", "bufs")

    def __init__(self, ap, bufs=None):
        self.ap = ap
        if bufs is None:
            bufs = [Buf()]
        elif isinstance(bufs, Buf):
            bufs = [bufs]
        self.bufs = bufs

    def v(self, ap):
        return T(ap, self.bufs)

    def __getitem__(self, k):
        return T(self.ap[k], self.bufs)


class Sched:
    ENG = ("pe", "act", "dve", "pool", "sp")

    def __init__(self, nc, es):
        self.nc = nc
        self.es = es
        self.h = {"pe": nc.tensor, "act": nc.scalar, "dve": nc.vector, "pool": nc.gpsimd, "sp": nc.sync}
        self.ops = {k: [] for k in self.ENG}
        self.sig = {k: [] for k in self.ENG}
        self.dma_cnt = {}
        self.dma_keys = []

    def new_dma_sem(self, key):
        self.dma_cnt[key] = 0
        self.dma_keys.append(key)

    def _collect(self, reads, writes):
        deps = {}

        def add(tok):
            if tok is None:
                return
            k, v = tok
            if deps.get(k, 0) < v:
                deps[k] = v
        for b in reads:
            add(b.w)
        for b in writes:
            add(b.w)
            for k, v in b.r.items():
                add((k, v))
        return deps

    def op(self, eng, fn, reads=(), writes=(), dma=None):
        deps = self._collect(reads, writes)
        idx = len(self.ops[eng]) + 1
        fdeps = {}
        for k, v in deps.items():
            if k == eng:
                if eng == "pe" or eng == "sp" or dma is not None:
                    continue
                if v < idx - SAME_ENGINE_WINDOW:
                    continue
            fdeps[k] = v
            if k in self.sig:
                self.sig[k][v - 1] = True
        self.ops[eng].append((fn, fdeps, dma))
        self.sig[eng].append(False)
        if dma is not None:
            self.dma_cnt[dma] += 1
            tok = (dma, self.dma_cnt[dma])
        else:
            tok = (eng, idx)
        for b in reads:
            k, v = tok
            if b.r.get(k, 0) < v:
                b.r[k] = v
        for b in writes:
            b.w = tok
            b.r = {}
        return tok

    def emit(self):
        nc = self.nc
        sem = {k: self.es.enter_context(nc.semaphore("s_" + k)) for k in self.ENG}
        for k in self.dma_keys:
            sem[k] = self.es.enter_context(nc.semaphore("d_" + k))
        val = {}
        for e in self.ENG:
            c = 0
            arr = []
            for s in self.sig[e]:
                if s:
                    c += 1
                arr.append(c)
            val[e] = arr
        n_ins = 0
        for e in self.ENG:
            h = self.h[e]
            waited = {}
            for i, (fn, deps, dma) in enumerate(self.ops[e]):
                for k, v in deps.items():
                    if k in val:
                        sv = val[k][v - 1]
                    else:
                        sv = 16 * v
                    if waited.get(k, 0) >= sv:
                        continue
                    h.wait_ge(sem[k], sv)
                    waited[k] = sv
                ins = fn(h)
                n_ins += 1
                if dma is not None:
                    ins.then_inc(sem[dma], 16)
                elif self.sig[e][i]:
                    ins.then_inc(sem[e], 1)
        self.sem = sem
        return n_ins


def _unit(W, cols, k0, k1):
    sub = W[k0 * 128:k1 * 128][:, cols]
    sub = sub.reshape(k1 - k0, 128, sub.shape[1]).transpose(1, 0, 2)
    return np.ascontiguousarray(sub).reshape(128, -1)


def _vec(v):
    return np.ascontiguousarray(v.reshape(-1, 128).T)


def w_in_unit_cols(l):
    units = []
    units.append(("lo0", np.arange(2560, 2688)))
    units.append(("lo1", np.arange(2688, 2816)))
    if l > 0:
        units.append(("lo2", np.arange(2816, 2848)))
    for j in range(4):
        units.append((f"r{j}", np.arange(1024 + j * 128, 1024 + (j + 1) * 128)))
        units.append((f"k{j}", np.arange(1536 + j * 128, 1536 + (j + 1) * 128)))
        units.append((f"v{j}", np.arange(2048 + j * 128, 2048 + (j + 1) * 128)))
    for i in range(4):
        units.append((f"ca{i}", np.arange(i * 128, (i + 1) * 128)))
        units.append((f"cg{i}", np.arange(512 + i * 128, 512 + (i + 1) * 128)))
    return units


def layer_units(l):
    out = []
    for name, cols in w_in_unit_cols(l):
        out.append(("in_" + name, 8 * len(cols)))
    for n in range(8):
        out.append((f"out{n}", 1024))
    for n in range(8):
        out.append((f"q{n}", 1024))
    for n in range(8):
        out.append((f"o{n}", 1024))
    for i in range(NFF):
        out.append((f"g{i}", 1024))
        out.append((f"u{i}", 1024))
    for n in range(8):
        out.append((f"d{n}a", 1024))
        out.append((f"d{n}b", 1024))
        out.append((f"d{n}c", 6 * 128))
    return out


def prep_layer_weights(l, inp):
    w_in = inp["w_in_first"] if l == 0 else inp["w_in_rest"][l - 1]
    parts = []
    for name, cols in w_in_unit_cols(l):
        parts.append(_unit(w_in, cols, 0, 8))
    for W in (inp["w_out"][l], inp["wq"][l], inp["wo"][l]):
        for n in range(8):
            parts.append(_unit(W, np.arange(n * 128, (n + 1) * 128), 0, 8))
    wgu = inp["w_gu"][l]
    for i in range(NFF):
        parts.append(_unit(wgu, np.arange(i * 128, (i + 1) * 128), 0, 8))
        parts.append(_unit(wgu, np.arange(DFF + i * 128, DFF + (i + 1) * 128), 0, 8))
    wd = inp["w_down"][l]
    for n in range(8):
        cols = np.arange(n * 128, (n + 1) * 128)
        parts.append(_unit(wd, cols, 0, 8))
        parts.append(_unit(wd, cols, 8, 16))
        parts.append(_unit(wd, cols, 16, 22))
    return np.ascontiguousarray(np.concatenate(parts, axis=1))


PC = {}
_o = 0
for _name, _n in (("g", 48), ("mu", 15), ("cw", 124), ("cb", 4), ("lng", 4), ("lnb", 4), ("w0", 4), ("a0", 4),
                  ("v0", 4), ("kk", 4), ("ka", 4), ("rk", 4), ("xg", 4), ("xb", 4)):
    PC[_name] = _o
    _o += _n
NPAR = _o


def prep_params(l, inp):
    P = np.zeros((128, NPAR), np.float32)
    g = inp["norm_gains"][l]
    for i in range(6):
        P[:, PC["g"] + i * 8: PC["g"] + (i + 1) * 8] = _vec(g[i])
    mu = inp["mu_first"] if l == 0 else inp["mu_rest"][l - 1]
    mu_p = np.zeros(15 * 128, np.float32)
    mu_p[:mu.shape[0]] = mu
    P[:, PC["mu"]:PC["mu"] + 15] = _vec(mu_p)
    cw = inp["conv_w"][l]
    P[:, PC["cw"]:PC["cw"] + 124] = np.ascontiguousarray(
        cw.reshape(CW, 4, 128).transpose(2, 1, 0)).reshape(128, 124)
    for name, key in (("cb", "conv_b"), ("lng", "conv_ln_g"), ("lnb", "conv_ln_b"), ("w0", "w0"), ("a0", "a0"),
                      ("kk", "k_k"), ("ka", "k_a"), ("xg", "lnx_g"), ("xb", "lnx_b")):
        P[:, PC[name]:PC[name] + 4] = _vec(inp[key][l])
    P[:, PC["rk"]:PC["rk"] + 4] = _vec(inp["r_k"][l].reshape(-1))
    if l > 0:
        P[:, PC["v0"]:PC["v0"] + 4] = _vec(inp["v0"][l - 1])
    return P


def prep_lora(l, inp):
    A = np.zeros((128, 3, 512), np.float32)
    A[0:64, 0] = inp["w_up"][l]
    A[64:128, 0] = inp["a_up"][l]
    A[:, 1] = inp["g_up"][l]
    if l > 0:
        A[0:32, 2] = inp["v_up"][l - 1]
    return A


CONST_W = 1412


def make_consts():
    C = np.zeros((128, CONST_W), np.float32)
    p = np.arange(128)
    h = p // 64
    j = p % 64
    same = (h[:, None] == h[None, :])
    C[:, 0:128] = np.eye(128)
    mus = (same & (j[:, None] < j[None, :])).astype(np.float32)
    C[:, 128:256] = mus
    C[:, 256:384] = -mus
    mls = (same & (j[None, :] < j[:, None])).astype(np.float32)
    C[:, 384:512] = -mls
    t = np.arange(64)
    mui = (j[:, None] <= t[None, :]).astype(np.float32)
    C[:, 512:576] = mui
    C[:, 576:640] = -mui
    C[:, 640:768] = same.astype(np.float32) / 64.0
    C[:, 768:896] = same.astype(np.float32)
    C[:, 896:1024] = 1.0 / 1024.0
    C[:, 1024:1152] = 1.0 / 512.0
    C[:, 1152:1280] = 1.0
    C[:, 1280:1408] = -np.eye(128)
    C[:, 1408] = 1e-6
    C[:, 1409] = 1e-5
    C[:, 1410] = 64e-5
    C[:, 1411] = 1e-24
    return C


def build(NT=SEQ // 256, NL=DEPTH, TT=256):
    NCH = TT // 64
    nc = bass.Bass("TRN2", target_bir_lowering=False)
    es = ExitStack()
    S = Sched(nc, es)

    xT = nc.dram_tensor("xT", [D, NT * TT], F32, kind="ExternalInput").ap()
    memT = nc.dram_tensor("memT", [D, MEM], F32, kind="ExternalInput").ap()
    memg = nc.dram_tensor("memg", [128, 8], F32, kind="ExternalInput").ap()
    consts_d = nc.dram_tensor("consts", [128, CONST_W], F32, kind="ExternalInput").ap()
    par_d = nc.dram_tensor("par", [NL, 128, NPAR], F32, kind="ExternalInput").ap()
    lora_d = nc.dram_tensor("lora", [NL, 128, 3 * 512], F32, kind="ExternalInput").ap()
    wkv_d = nc.dram_tensor("wkvu", [NL, 128, 16 * 1024], F32, kind="ExternalInput").ap()
    lunits = [layer_units(l) for l in range(NL)]
    wl_d = [nc.dram_tensor(f"wl{l}", [128, sum(n for _, n in lunits[l])], F32, kind="ExternalInput").ap()
            for l in range(NL)]
    outT = nc.dram_tensor("outT", [D, NT * TT], F32, kind="ExternalOutput").ap()
    wlb_d = [nc.dram_tensor(f"wlb{l}", [128, sum(n for _, n in lunits[l])], BF16).ap() for l in range(NL)]
    if DEBUG:
        dbg_d = nc.dram_tensor("dbg", [128, KC * TT], F32, kind="ExternalOutput").ap()

    def sb(name, shape, dt):
        return es.enter_context(nc.sbuf_tensor(name, shape, dt))

    def ps_t(name, shape, dt):
        return es.enter_context(nc.psum_tensor(name, shape, dt))

    X = sb("X", [128, KC, TT], F32)
    Xc = [T(X[:, c, :]) for c in range(KC)]
    Hh = sb("H", [128, KC, TT], BF16)
    Hc = [T(Hh[:, c, :]) for c in range(KC)]
    MIXb = sb("MIX", [128, KC, TT], BF16)
    MIXc = [T(MIXb[:, c, :]) for c in range(KC)]
    Y = sb("Y", [128, KC, TT], F32)
    Yc = [T(Y[:, c, :]) for c in range(KC)]
    KT = [sb(f"KT{l}", [128, KC, MEM], BF16) for l in range(NL)]
    KTt = [T(KT[l][:]) for l in range(NL)]
    VM = [sb(f"VM{l}", [128, 2, D], BF16) for l in range(NL)]
    VMt = [T(VM[l][:]) for l in range(NL)]
    RING = sb("ring", [128, NSLOT, 1024], BF16)
    ring = [T(RING[:, s, :]) for s in range(NSLOT)]
    VF = sb("VF", [128, 4, TT], F32)
    VFc = [T(VF[:, j, :]) for j in range(4)]
    HS = [[T(sb(f"HS{l}_{j}", [128, 128], F32)[:]) for j in range(4)] for l in range(NL)]
    HSb = [[[T(sb(f"HSb{l}_{j}_{k}", [128, 128], BF16)[:]) for k in range(2)] for j in range(4)] for l in range(NL)]
    hsb_par = [[0] * 4 for _ in range(NL)]
    PAR = [T(sb(f"PAR{l}", [128, NPAR], F32)[:]) for l in range(NL)]
    LORA = [T(sb(f"LORA{l}", [128, 3, 512], BF16)[:]) for l in range(NL)]
    CH = [T(sb(f"CH{l}", [128, 4, CW - 1], F32)[:]) for l in range(NL)]
    SH = [T(sb(f"SH{l}", [128, 15], F32)[:]) for l in range(NL)]
    CF = T(sb("CF", [128, CONST_W], F32)[:])
    CB = T(sb("CB", [128, CONST_W], BF16)[:])

    def cf(a, b):
        return CF[:, a:b]

    def cb(a, b):
        return CB[:, a:b]
    IDb = cb(0, 128)
    MUS, NMUS, NMLS = cf(128, 256), cf(256, 384), cf(384, 512)
    MUI, NMUI = cf(512, 576), cf(576, 640)
    IDf = cf(0, 128)
    BO64b, BO1b = cb(640, 768), cb(768, 896)
    O1024b, O512b, O1b = cb(896, 1024), cb(1024, 1152), cb(1152, 1280)

    RSTD = T(sb("RSTD", [128, TT], F32)[:])
    SQ = [T(sb(f"SQ{i}", [128, TT], BF16)[:]) for i in range(2)]
    sq_i = [0]
    PT = [T(sb(f"PT{i}", [128, TT + 1], F32)[:]) for i in range(3)]
    pt_i = [0]
    TMP = [T(sb(f"TMP{i}", [128, TT], F32)[:]) for i in range(4)]
    tmp_i = [0]

    def tmp():
        t = TMP[tmp_i[0] % len(TMP)]
        tmp_i[0] += 1
        return t
    LOb = T(sb("LOb", [128, TT], BF16)[:])
    Gb = T(sb("Gb", [128, TT], BF16)[:])
    VRb = T(sb("VRb", [32, TT], BF16)[:])
    RAW = [[T(sb(f"RAW{i}_{q}", [128, TT], F32)[:]) for q in range(3)] for i in range(2)]
    RAW.append([Yc[0], Yc[1], Yc[2]])
    RAW.append([Yc[3], Yc[4], Yc[5]])
    P0 = sb("P0", [128, NCH, 96], F32)
    P1 = sb("P1", [128, NCH, 96], F32)
    P0t, P1t = T(P0[:]), T(P1[:])
    At = T(sb("A_t", [128, TT], F32)[:])
    KKt = T(sb("KK_t", [128, TT], F32)[:])
    Kpt = T(sb("Kp_t", [128, TT], F32)[:])
    Bvt = T(sb("Bv_t", [128, TT], F32)[:])
    EX = [T(sb(f"EX{i}", [128, NCH, 64], F32)[:]) for i in range(4)]
    GCt = [T(sb(f"GC{i}", [128, NCH], F32)[:]) for i in range(2)]
    NSET = 2
    EXPN = ("KKe", "KTe", "BTe", "KHe", "BHe", "Ve")
    EXP = [{n: T(sb(f"{n}{i}", [128, NCH, 128], BF16)[:]) for n in EXPN} for i in range(NSET)]
    RT = [T(sb(f"Rt{i}", [128, TT], BF16)[:]) for i in range(NSET)]
    Gt = [T(sb(f"G_t{i}", [128, TT], F32)[:]) for i in range(NSET)]
    BON = [T(sb(f"BON{i}", [128, TT], F32)[:]) for i in range(NSET)]
    YR = [T(sb(f"YR{i}", [128, TT], F32)[:]) for i in range(NSET)]
    MATN = ("LkkT", "M1", "N1", "Sa", "Sb", "Ma", "Mb", "Na", "Nb", "Vbd", "KHbd", "BHbdn", "KKbd", "X1", "TKT", "Usb")
    NMSET = NCH
    MATS = []
    for i in range(min(2, NMSET)):
        d_ = {n: T(sb(f"m_{n}{i}", [128, 128], BF16)[:]) for n in MATN}
        d_["MrkT"] = T(sb(f"m_MrkT{i}", [128, 64], BF16)[:])
        d_["MrbTn"] = T(sb(f"m_MrbTn{i}", [128, 64], BF16)[:])
        MATS.append(d_)
    mset_i = [0]
    HG = sb("HG", [128, 4, CW - 1 + TT], F32)
    HGc = [T(HG[:, i, :]) for i in range(4)]
    ACC = sb("ACC", [128, 4, TT], F32)
    ACCc = [T(ACC[:, i, :]) for i in range(4)]
    MEANt = T(sb("MEANt", [128, TT], F32)[:])
    VARt = T(sb("VARt", [128, TT], F32)[:])
    Q = sb("Q", [128, KC, TT], BF16)
    Qc = [T(Q[:, c, :]) for c in range(KC)]
    E = [T(sb(f"E{i}", [128, 2, TT], BF16)[:]) for i in range(2)]
    RD = [T(sb(f"RD{i}", [128, TT], F32)[:]) for i in range(2)]
    Oo = sb("Oo", [128, KC, TT], BF16)
    Oc = [T(Oo[:, c, :]) for c in range(KC)]
    AV = sb("AV", [128, NFF, TT], BF16)
    AVc = [T(AV[:, i, :], [Buf() for _ in range(TT // 128)]) for i in range(NFF)]
    _slots = [(i, k) for i in range(NFF) for k in range(TT // 128)]
    _si = 0
    for i in range(2, NMSET):
        d_ = {}
        for n in list(MATN) + ["MrkT", "MrbTn"]:
            ci, k = _slots[_si]
            _si += 1
            wdt = 64 if n in ("MrkT", "MrbTn") else 128
            d_[n] = T(AV[:, ci, k * 128:k * 128 + wdt], [AVc[ci].bufs[k]])
        MATS.append(d_)
    MN = T(Hh[:, :, 0:MEM], [b for t in Hc for b in t.bufs])
    MF = T(Y[:, :, 0:MEM], [b for t in Yc for b in t.bufs])
    MG = T(sb("MG", [128, 8], F32)[:])
    VTt = T(sb("VTt", [128, MEM], BF16)[:])

    PSB = [ps_t(f"psb{i}", [128, 512], F32) for i in range(8)]
    PSBT = [T(PSB[b][:]) for b in range(8)]
    pb_i = [0]

    def pbig():
        t = PSBT[pb_i[0] % 8]
        pb_i[0] += 1
        return t[:, 0:TT]

    def psm():
        t = PSBT[pb_i[0] % 8]
        pb_i[0] += 1
        return t[:, 0:128]

    def bufs_of(*ts):
        out = []
        for t in ts:
            if isinstance(t, T):
                out.extend(t.bufs)
        return out

    def apof(x):
        return x.ap if isinstance(x, T) else x

    def MM(out, lhsT, rhs, start=True, stop=True):
        S.op("pe", lambda e: e.matmul(out.ap, lhsT=lhsT.ap, rhs=rhs.ap, start=start, stop=stop),
             bufs_of(lhsT, rhs), out.bufs)

    def ACT(out, in_, func, bias=None, scale=1.0):
        kw = {}
        if bias is not None:
            kw["bias"] = apof(bias)
        S.op("act", lambda e: e.activation(out=out.ap, in_=in_.ap, func=func, scale=apof(scale), **kw),
             bufs_of(in_, bias, scale), out.bufs)

    def TTo(out, in0, in1, op, eng="dve"):
        S.op(eng, lambda e: e.tensor_tensor(out=out.ap, in0=in0.ap, in1=in1.ap, op=op),
             bufs_of(in0, in1), out.bufs)

    def TS(out, in0, s1, op0, s2=None, op1=None, eng="dve"):
        if op1 is None:
            S.op(eng, lambda e: e.tensor_scalar(out=out.ap, in0=in0.ap, scalar1=apof(s1), scalar2=None, op0=op0),
                 bufs_of(in0, s1), out.bufs)
        else:
            S.op(eng, lambda e: e.tensor_scalar(out=out.ap, in0=in0.ap, scalar1=apof(s1), scalar2=apof(s2),
                                                op0=op0, op1=op1),
                 bufs_of(in0, s1, s2), out.bufs)

    def STT(out, in0, sc, in1, op0, op1, eng="dve"):
        S.op(eng, lambda e: e.scalar_tensor_tensor(out=out.ap, in0=in0.ap, scalar=apof(sc), in1=in1.ap,
                                                   op0=op0, op1=op1),
             bufs_of(in0, sc, in1), out.bufs)

    def CP(out, in_, eng="dve"):
        if eng == "act":
            ACT(out, in_, AF.Copy)
        else:
            S.op(eng, lambda e: e.tensor_copy(out=out.ap, in_=in_.ap), in_.bufs, out.bufs)

    def MEMSET(t, val, eng="dve"):
        S.op(eng, lambda e: e.memset(t.ap, val), (), t.bufs)

    def DMA(eng, out, in_ap, key):
        S.op(eng, lambda e: e.dma_start(out=out.ap, in_=in_ap), (), out.bufs, dma=key)

    def DMAOUT(eng, out_ap, in_, key):
        S.op(eng, lambda e: e.dma_start(out=out_ap, in_=in_.ap), in_.bufs, (), dma=key)

    EPSC = {1e-6: 1408, 1e-5: 1409, 64e-5: 1410, 1e-24: 1411}

    def RSQRT(out, in_, eps):
        c = EPSC[eps]
        ACT(out, in_, AF.Sqrt, bias=CF[:, c:c + 1])
        S.op("dve", lambda e: e.reciprocal(out=out.ap, in_=out.ap), out.bufs, out.bufs)

    evac_i = [0]

    def EVAC(out, in_):
        evac_i[0] += 1
        if evac_i[0] % 4:
            CP(out, in_, "act")
        else:
            CP(out, in_, "dve")

    for s in range(NSLOT):
        S.new_dma_sem(f"w{s}")
    wseq = []
    ubufs = {}
    for s_ in range(NSLOT):
        S.new_dma_sem(f"ws{s_}")
    for l in range(NL):
        for n in range(16):
            wseq.append((wkv_d[l, :, n * 1024:(n + 1) * 1024], 1024, f"kv{l}_{n}", None))
    for t in range(NT):
        for l in range(NL):
            off = 0
            for name, ncols in lunits[l]:
                ub = ubufs.setdefault((l, name), Buf())
                wseq.append((wl_d[l][:, off:off + ncols], ncols, f"L{l}_{name}",
                             (wlb_d[l][:, off:off + ncols], ub, t)))
                off += ncols
    wst = {"issue": 0, "use": 0}

    def wget(expect):
        while wst["issue"] < min(len(wseq), wst["use"] + NSLOT):
            u = wst["issue"]
            ap, ncols, _, binfo = wseq[u]
            slot = u % NSLOT
            dst = ring[slot][:, 0:ncols]
            if binfo is None or (binfo[2] == 0 and NT == 1):
                DMA("pool", dst, ap, f"w{slot}")
            elif binfo[2] == 0:
                DMA("pool", dst, ap, f"w{slot}")
                bap, ub, _t = binfo
                S.op("sp", lambda e, o=bap, i=dst.ap: e.dma_start(out=o, in_=i), dst.bufs, [ub], dma=f"ws{slot}")
            else:
                bap, ub, _t = binfo
                S.op("sp", lambda e, o=dst.ap, i=bap: e.dma_start(out=o, in_=i), [ub], dst.bufs, dma=f"w{slot}")
            wst["issue"] += 1
        u = wst["use"]
        assert wseq[u][2] == expect, (wseq[u][2], expect)
        wst["use"] += 1
        return ring[u % NSLOT]

    def setup_dma(eng, out, in_ap, key):
        S.new_dma_sem(key)
        DMA(eng, out, in_ap, key)
    for k in ("xin", "xout"):
        S.new_dma_sem(k)
    setup_dma("sp", CF, consts_d[:, :], "cst")
    setup_dma("pool", CB, consts_d[:, :], "cstb")
    for l in range(NL):
        setup_dma("sp", PAR[l], par_d[l], f"par{l}")
    for l in range(NL):
        setup_dma("pool", LORA[l], lora_d[l].rearrange("p (a b) -> p a b", a=3), f"lora{l}")
    setup_dma("sp", MF, memT.rearrange("(c p) m -> p c m", p=128), "mem")
    setup_dma("sp", MG, memg[:, :], "memg")
    for l in range(NL):
        for j in range(4):
            MEMSET(HS[l][j], 0.0)
            MEMSET(HSb[l][j][0], 0.0)
            MEMSET(HSb[l][j][1], 0.0)
        MEMSET(CH[l], 0.0)
        MEMSET(SH[l], 0.0)
    for i in range(NSET):
        for n in EXPN:
            MEMSET(EXP[i][n], 0.0)
    MEMSET(P0t, 0.0)
    MEMSET(P1t, 0.0)

    def sqtile():
        t = SQ[sq_i[0] % 2]
        sq_i[0] += 1
        return t

    def rms_stats(src_list, out_rstd, ncol, ones_b, eps):
        p = pbig()
        n = len(src_list)
        for i, s in enumerate(src_list):
            q = sqtile()
            ACT(q[:, 0:ncol], s, AF.Square)
            MM(p[:, 0:ncol], ones_b, q[:, 0:ncol], start=(i == 0), stop=(i == n - 1))
        RSQRT(out_rstd, p[:, 0:ncol], eps)

    MFc = [MF[:, c, :] for c in range(KC)]
    MRS = T(sb("MRS", [128, MEM], F32)[:])
    rms_stats(MFc, MRS, MEM, O1024b, 1e-6)
    for c in range(KC):
        STT(MN[:, c, :], MF[:, c, :], MG[:, c:c + 1], MRS, ALU.mult, ALU.mult)
    for l in range(NL):
        for n in range(16):
            w = wget(f"kv{l}_{n}")
            p = pbig()
            for kc in range(KC):
                MM(p[:, 0:MEM], w[:, kc * 128:(kc + 1) * 128], MN[:, kc, :], start=(kc == 0), stop=(kc == KC - 1))
            if n < 8:
                EVAC(KTt[l][:, n, :], p[:, 0:MEM])
            else:
                EVAC(VTt, p[:, 0:MEM])
                for mc in range(2):
                    q = psm()
                    MM(q, VTt[:, mc * 128:(mc + 1) * 128], IDb)
                    EVAC(VMt[l][:, mc, (n - 8) * 128:(n - 7) * 128], q)

    def pre_norm(l, gi):
        rms_stats(Xc, RSTD, TT, O1024b, 1e-6)
        for c in range(KC):
            col = PC["g"] + gi * 8 + c
            STT(Hc[c], Xc[c], PAR[l][:, col:col + 1], RSTD, ALU.mult, ALU.mult)

    def post_norm(l, gi):
        rms_stats(Yc, RSTD, TT, O1024b, 1e-6)
        for c in range(KC):
            col = PC["g"] + gi * 8 + c
            t = tmp()
            STT(t, Yc[c], PAR[l][:, col:col + 1], RSTD, ALU.mult, ALU.mult)
            TTo(Xc[c], Xc[c], t, ALU.add)

    def linear8(l, prefix, src, nout, consume):
        for n in range(nout):
            w = wget(f"L{l}_{prefix}{n}")
            p = pbig()
            for kc in range(KC):
                MM(p, w[:, kc * 128:(kc + 1) * 128], src[kc], start=(kc == 0), stop=(kc == KC - 1))
            consume(n, p)

    def rwkv_pre(l, j, raw, si):
        R, K, V = raw
        P = PAR[l]
        ex = EXP[si]
        pc = lambda name: P[:, PC[name] + j:PC[name] + j + 1]
        lora = LORA[l]
        jc = slice(j * 128, (j + 1) * 128)
        v3 = lambda t: t.v(t.ap.rearrange("p (c t) -> p c t", t=64))
        p = pbig()
        MM(p, lora[0:64, 0, jc], LOb[0:64, :])
        t1 = tmp()
        ACT(t1, p, AF.Sigmoid, bias=pc("w0"))
        LG = P0t[:, :, 32:96]
        TS(LG, v3(t1), -DECAY_SCALE, ALU.mult)
        yield
        p = pbig()
        MM(p, lora[64:128, 0, jc], LOb[64:128, :])
        ACT(At, p, AF.Sigmoid, bias=pc("a0"))
        p = pbig()
        MM(p, lora[:, 1, jc], Gb)
        CP(Gt[si], p, "act")
        yield
        if l == 0:
            CP(VFc[j], V, "dve")
        else:
            p = pbig()
            MM(p, lora[0:32, 2, jc], VRb[0:32, :])
            t2 = tmp()
            ACT(t2, p, AF.Sigmoid, bias=pc("v0"))
            t3 = tmp()
            TTo(t3, VFc[j], V, ALU.subtract)
            TTo(t3, t3, t2, ALU.mult)
            TTo(V, V, t3, ALU.add)
        yield
        TS(KKt, K, pc("kk"), ALU.mult)
        q = sqtile()
        ACT(q, KKt, AF.Square)
        p = pbig()
        MM(p, BO1b, q)
        t4 = tmp()
        RSQRT(t4, p, 1e-24)
        TTo(KKt, KKt, t4, ALU.mult)
        yield
        t5 = tmp()
        TS(t5, At, pc("ka"), ALU.mult, pc("ka"), ALU.subtract)
        STT(Kpt, t5, 1.0, K, ALU.add, ALU.mult)
        TTo(Bvt, KKt, At, ALU.mult)
        yield
        q = sqtile()
        STT(q, R, pc("rk"), Kpt, ALU.mult, ALU.mult)
        p = pbig()
        MM(p, BO1b, q)
        TTo(BON[si], p, V, ALU.mult)
        yield
        src, dst = P0t, P1t
        for s_ in (1, 2, 4, 8, 16, 32):
            TTo(dst[:, :, 32:96], src[:, :, 32:96], src[:, :, 32 - s_:96 - s_], ALU.add)
            src, dst = dst, src
            yield
        assert src is P0t
        LGP = P0t[:, :, 31:95]
        ACT(EX[0], LG, AF.Exp)
        ACT(EX[1], LGP, AF.Exp)
        ACT(EX[2], LG, AF.Exp, scale=-1.0)
        gc = GCt[si]
        ACT(gc, P0t[:, :, 95], AF.Exp)
        yield
        TTo(EX[3], EX[2], gc.v(gc.ap.unsqueeze(2).to_broadcast([128, NCH, 64])), ALU.mult)
        TTo(RT[si], R, EX[0].v(EX[0].ap.rearrange("p c t -> p (c t)")), ALU.mult)
        yield
        for h in range(2):
            ph = slice(h * 64, (h + 1) * 64)
            fh = slice(h * 64, (h + 1) * 64)
            TTo(ex["KKe"][ph, :, fh], v3(KKt)[ph], EX[1][ph], ALU.mult)
            TTo(ex["KTe"][ph, :, fh], v3(Kpt)[ph], EX[2][ph], ALU.mult)
            yield
            TTo(ex["BTe"][ph, :, fh], v3(Bvt)[ph], EX[2][ph], ALU.mult)
            TTo(ex["KHe"][ph, :, fh], v3(Kpt)[ph], EX[3][ph], ALU.mult)
            yield
            TTo(ex["BHe"][ph, :, fh], v3(Bvt)[ph], EX[3][ph], ALU.mult)
            CP(ex["Ve"][ph, :, fh], v3(V)[ph], "dve")
            yield

    def rwkv_chunks(l, j, si):
        ex = EXP[si]
        ms = [MATS[c % NMSET] for c in range(NCH)]
        assert NMSET >= NCH
        opnd = []
        for c in range(NCH):
            opnd.append([ex[n][:, c, :] for n in EXPN] + [RT[si][:, c * 64:(c + 1) * 64]])
        for c in range(NCH):
            m = ms[c]
            KKe, KTe, BTe, KHe, BHe, Ve, Rc = opnd[c]
            p = psm(); MM(p, KTe, KKe); TTo(m["LkkT"], p, MUS, ALU.mult)
            p = psm(); MM(p, BTe, KKe); TTo(m["M1"], p, NMUS, ALU.mult)
            p = psm(); MM(p, KKe, BTe); TTo(m["N1"], p, NMLS, ALU.mult)
            yield
            p = psm(); MM(p[:, 0:64], KTe, Rc); TTo(m["MrkT"], p[:, 0:64], MUI, ALU.mult)
            p = psm(); MM(p[:, 0:64], BTe, Rc); TTo(m["MrbTn"], p[:, 0:64], NMUI, ALU.mult)
            TTo(m["Sa"], m["M1"], IDf, ALU.add)
            yield
            p = psm(); MM(p, Ve, IDb); EVAC(m["Vbd"], p)
            p = psm(); MM(p, KHe, IDb); EVAC(m["KHbd"], p)
            yield
            p = psm(); MM(p, BHe, cb(1280, 1408)); EVAC(m["BHbdn"], p)
            p = psm(); MM(p, KKe, IDb); EVAC(m["KKbd"], p)
            yield
        cur = [[ms[c]["M1"], ms[c]["N1"], ms[c]["Sa"], ms[c]["Ma"], ms[c]["Na"], ms[c]["Sb"]] for c in range(NCH)]
        for lvl in range(5):
            last = (lvl == 4)
            for c in range(NCH):
                m = ms[c]
                Mc, Nc, Sc, Mn, Nn, Sn = cur[c]
                if not last:
                    p = psm(); MM(p, Nc, Mc); EVAC(Mn, p)
                p = psm(); MM(p, Mc, Nc); EVAC(Nn, p)
                yield
            for c in range(NCH):
                m = ms[c]
                Mc, Nc, Sc, Mn, Nn, Sn = cur[c]
                p = psm()
                MM(p, Nn, Sc, start=True, stop=False)
                MM(p, IDb, Sc, start=False, stop=True)
                EVAC(Sn, p)
                if lvl == 0:
                    cur[c] = [Mn, Nn, Sn, m["Mb"], m["Nb"], m["Sa"]]
                else:
                    cur[c] = [Mn, Nn, Sn, Mc, Nc, Sc]
                yield
        for c in range(NCH):
            m = ms[c]
            TT_ = cur[c][2]
            p = psm(); MM(p, m["LkkT"], m["Vbd"]); EVAC(m["X1"], p)
            p = psm(); MM(p, m["KKbd"], TT_); EVAC(m["TKT"], p)
            yield
        hs = HS[l][j]
        for c in range(NCH):
            m = ms[c]
            TT_ = cur[c][2]
            Rc = opnd[c][6]
            gcol = GCt[si][:, c:c + 1]
            hb_old = HSb[l][j][hsb_par[l][j]]
            hb_new = HSb[l][j][1 - hsb_par[l][j]]
            hsb_par[l][j] = 1 - hsb_par[l][j]
            p = psm()
            MM(p, TT_, m["X1"], start=True, stop=False)
            MM(p, m["TKT"], hb_old, start=False, stop=True)
            CP(m["Usb"], p, "act")
            p = psm()
            MM(p, m["KHbd"], m["Vbd"], start=True, stop=False)
            MM(p, m["BHbdn"], m["Usb"], start=False, stop=True)
            STT(hs, hs, gcol, p, ALU.mult, ALU.add)
            CP(hb_new, hs, "act")
            py = psm()
            MM(py[:, 0:64], hb_old, Rc, start=True, stop=False)
            MM(py[:, 0:64], m["Vbd"], m["MrkT"], start=False, stop=False)
            MM(py[:, 0:64], m["Usb"], m["MrbTn"], start=False, stop=True)
            CP(YR[si][:, c * 64:(c + 1) * 64], py[:, 0:64], "act")
            yield

    def rwkv_post(l, j, si):
        P = PAR[l]
        pc = lambda name: P[:, PC[name] + j:PC[name] + j + 1]
        yr = YR[si]
        q = sqtile()
        CP(q, yr, "act")
        pm = pbig()
        MM(pm, BO64b, q)
        q2 = sqtile()
        ACT(q2, yr, AF.Square)
        pe2 = pbig()
        MM(pe2, BO64b, q2)
        mean = tmp()
        CP(mean, pm, "act")
        m2 = tmp()
        TTo(m2, mean, mean, ALU.mult)
        var = tmp()
        TTo(var, pe2, m2, ALU.subtract)
        RSQRT(var, var, 64e-5)
        t1 = tmp()
        TTo(t1, yr, mean, ALU.subtract)
        TTo(t1, t1, var, ALU.mult)
        TS(t1, t1, pc("xg"), ALU.mult, pc("xb"), ALU.add)
        TTo(t1, t1, BON[si], ALU.add)
        TTo(MIXc[4 + j], t1, Gt[si], ALU.mult)

    def conv_task(l):
        P = PAR[l]
        for jj in range(CW):
            for i in range(4):
                wcol = P[:, PC["cw"] + i * CW + jj:PC["cw"] + i * CW + jj + 1]
                if jj == 0:
                    TS(ACCc[i], HGc[i][:, 0:TT], wcol, ALU.mult, P[:, PC["cb"] + i:PC["cb"] + i + 1], ALU.add,
                       eng=CONV_ENG)
                else:
                    STT(ACCc[i], HGc[i][:, jj:jj + TT], wcol, ACCc[i], ALU.mult, ALU.add, eng=CONV_ENG)
            yield
        pm = pbig()
        pe2 = pbig()
        for i in range(4):
            q = sqtile()
            CP(q, ACCc[i], "act")
            MM(pm, O512b, q, start=(i == 0), stop=(i == 3))
            q2 = sqtile()
            ACT(q2, ACCc[i], AF.Square)
            MM(pe2, O512b, q2, start=(i == 0), stop=(i == 3))
        CP(MEANt, pm, "act")
        m2 = tmp()
        TTo(m2, MEANt, MEANt, ALU.mult)
        var = VARt
        TTo(var, pe2, m2, ALU.subtract)
        RSQRT(var, var, 1e-5)
        yield
        for i in range(4):
            t1 = tmp()
            TTo(t1, ACCc[i], MEANt, ALU.subtract)
            TTo(t1, t1, var, ALU.mult)
            ACT(MIXc[i], t1, AF.Silu, bias=P[:, PC["lnb"] + i:PC["lnb"] + i + 1],
                scale=P[:, PC["lng"] + i:PC["lng"] + i + 1])
            yield

    def mixer(l):
        P = PAR[l]
        pre_norm(l, 0)
        for i in range(4):
            CP(HGc[i][:, 0:CW - 1], CH[l][:, i, :], "act")
        names = [n for n, _ in w_in_unit_cols(l)]
        state = {}
        for name in names:
            w = wget(f"L{l}_in_{name}")
            ncol = 32 if name == "lo2" else 128
            p = pbig()
            for kc in range(KC):
                MM(p[0:ncol, :], w[:, kc * ncol:(kc + 1) * ncol], Hc[kc], start=(kc == 0), stop=(kc == KC - 1))
            kind = name[0]
            if kind in ("l", "r", "k", "v"):
                if kind == "l":
                    midx = 12 + int(name[2])
                else:
                    midx = {"r": 0, "k": 4, "v": 8}[kind] + int(name[1])
                pt = PT[pt_i[0] % len(PT)]
                pt_i[0] += 1
                rows = slice(0, ncol)
                CP(pt[rows, 0:1], SH[l][rows, midx:midx + 1], "act")
                CP(pt[rows, 1:TT + 1], p[rows, :], "act")
                d = tmp()
                TTo(d[rows], pt[rows, 0:TT], pt[rows, 1:TT + 1], ALU.subtract)
                CP(SH[l][rows, midx:midx + 1], pt[rows, TT:TT + 1], "act")
                mucol = P[rows, PC["mu"] + midx:PC["mu"] + midx + 1]
                if kind == "l":
                    sh = tmp()
                    STT(sh[rows], d[rows], mucol, pt[rows, 1:TT + 1], ALU.mult, ALU.add)
                    if name == "lo0":
                        ACT(LOb[0:64, :], sh[0:64, :], AF.Tanh)
                        CP(LOb[64:128, :], sh[64:128, :], "act")
                    elif name == "lo1":
                        ACT(Gb, sh, AF.Sigmoid)
                    else:
                        CP(VRb[0:32, :], sh[0:32, :], "act")
                else:
                    j = int(name[1])
                    dst = RAW[j][{"r": 0, "k": 1, "v": 2}[kind]]
                    STT(dst, d, mucol, pt[:, 1:TT + 1], ALU.mult, ALU.add)
            else:
                i = int(name[2])
                if name[1] == "a":
                    state["pa"] = p
                else:
                    sg = tmp()
                    ACT(sg, p, AF.Sigmoid)
                    TTo(HGc[i][:, CW - 1:CW - 1 + TT], state["pa"], sg, ALU.mult)
        for i in range(4):
            CP(CH[l][:, i, :], HGc[i][:, TT:TT + CW - 1], "act")
        prog = {"pre": 0, "chunks": 0}

        def thread_pre():
            for j in range(4):
                while prog["chunks"] < j - NSET + 1:
                    yield
                yield from rwkv_pre(l, j, RAW[j], j % NSET)
                prog["pre"] = j + 1
                yield

        def thread_chunks():
            for j in range(4):
                while prog["pre"] < j + 1:
                    yield
                yield from rwkv_chunks(l, j, j % NSET)
                prog["chunks"] = j + 1
                rwkv_post(l, j, j % NSET)
                yield

        active = [thread_pre(), thread_chunks()]
        conv = conv_task(l)
        conv_done = False
        rounds = 0
        while active or not conv_done:
            for g in list(active):
                try:
                    next(g)
                except StopIteration:
                    active.remove(g)
            rounds += 1
            if not conv_done and prog["pre"] >= 1 and (rounds % CONV_EVERY == 0 or not active):
                try:
                    next(conv)
                except StopIteration:
                    conv_done = True
        if DEBUG and l == NL - 1 and dbg_state["t"] == 0:
            DBGT = T(sb("DBGT", [128, KC, TT], F32)[:])
            for c in range(KC):
                CP(DBGT[:, c, :], MIXc[c], "dve")
            S.new_dma_sem("dbg")
            DMAOUT("sp", dbg_d.rearrange("p (c t) -> p c t", c=KC), DBGT, "dbg")
        linear8(l, "out", MIXc, 8, lambda n, p: CP(Yc[n], p, "act"))
        post_norm(l, 1)

    def xattn(l):
        pre_norm(l, 2)
        linear8(l, "q", Hc, 8, lambda n, p: ACT(Qc[n], p, AF.Copy, scale=1.0 / 16.0))
        for h in range(4):
            e = E[h % 2]
            rd = RD[h % 2]
            for mc in range(2):
                p = pbig()
                for dc in range(2):
                    MM(p, KTt[l][:, 2 * h + dc, mc * 128:(mc + 1) * 128], Qc[2 * h + dc], start=(dc == 0), stop=(dc == 1))
                ACT(e[:, mc, :], p, AF.Exp)
            p = pbig()
            for mc in range(2):
                MM(p, O1b, e[:, mc, :], start=(mc == 0), stop=(mc == 1))
            S.op("dve", lambda en, o=rd.ap, i=p.ap: en.reciprocal(out=o, in_=i), p.bufs, rd.bufs)
            for dc in range(2):
                p = pbig()
                for mc in range(2):
                    MM(p, VMt[l][:, mc, (2 * h + dc) * 128:(2 * h + dc + 1) * 128], e[:, mc, :],
                       start=(mc == 0), stop=(mc == 1))
                TTo(Oc[2 * h + dc], p, rd, ALU.mult)
        linear8(l, "o", Oc, 8, lambda n, p: CP(Yc[n], p, "act"))
        post_norm(l, 3)

    def ffn(l):
        pre_norm(l, 4)
        for i in range(NFF):
            wg = wget(f"L{l}_g{i}")
            pg = pbig()
            for kc in range(KC):
                MM(pg, wg[:, kc * 128:(kc + 1) * 128], Hc[kc], start=(kc == 0), stop=(kc == KC - 1))
            wu = wget(f"L{l}_u{i}")
            pu = pbig()
            for kc in range(KC):
                MM(pu, wu[:, kc * 128:(kc + 1) * 128], Hc[kc], start=(kc == 0), stop=(kc == KC - 1))
            sg = tmp()
            ACT(sg, pg, AF.Silu)
            TTo(AVc[i], pu, sg, ALU.mult)
        for n in range(8):
            p = pbig()
            k = 0
            for part, nk in (("a", 8), ("b", 8), ("c", 6)):
                w = wget(f"L{l}_d{n}{part}")
                for kk in range(nk):
                    MM(p, w[:, kk * 128:(kk + 1) * 128], AVc[k], start=(k == 0), stop=(k == NFF - 1))
                    k += 1
            CP(Yc[n], p, "act")
        post_norm(l, 5)

    xT_v = xT.rearrange("(c p) t -> p c t", p=128)
    outT_v = outT.rearrange("(c p) t -> p c t", p=128)
    Xall = T(X[:], [b for t in Xc for b in t.bufs])
    dbg_state = {"t": 0}
    for t in range(NT):
        dbg_state["t"] = t
        DMA("sp", Xall, xT_v[:, :, t * TT:(t + 1) * TT], "xin")
        for l in range(NL):
            last = (l == NL - 1)
            mixer(l)
            if last and STAGE < 2:
                for n in range(8):
                    wget(f"L{l}_q{n}")
                for n in range(8):
                    wget(f"L{l}_o{n}")
            else:
                xattn(l)
            if last and STAGE < 3:
                for i in range(NFF):
                    wget(f"L{l}_g{i}")
                    wget(f"L{l}_u{i}")
                for n in range(8):
                    for part in "abc":
                        wget(f"L{l}_d{n}{part}")
            else:
                ffn(l)
        DMAOUT("sp", outT_v[:, :, t * TT:(t + 1) * TT], Xall, "xout")
    assert wst["use"] == len(wseq), (wst["use"], len(wseq))
    n_ins = S.emit()
    nc.sync.wait_ge(S.sem["xout"], 16 * S.dma_cnt["xout"])
    return nc, es, n_ins


_CACHE = {}


def prep_inputs(inp, NT, NL, TT):
    x = np.asarray(inp["x"], np.float32)
    mem = np.asarray(inp["mem"], np.float32)
    B = x.shape[0]
    Tn = NT * TT
    shared = {
        "memg": _vec(np.asarray(inp["mem_norm_g"], np.float32)),
        "consts": make_consts(),
        "par": np.stack([prep_params(l, inp) for l in range(NL)]),
        "lora": np.stack([prep_lora(l, inp).reshape(128, 1536) for l in range(NL)]),
    }
    wkv = []
    for l in range(NL):
        W = np.asarray(inp["wkv"][l], np.float32)
        wkv.append(np.concatenate([_unit(W, np.arange(n * 128, (n + 1) * 128), 0, 8) for n in range(16)], axis=1))
    shared["wkvu"] = np.ascontiguousarray(np.stack(wkv))
    for l in range(NL):
        shared[f"wl{l}"] = prep_layer_weights(l, inp)
    maps = []
    for b in range(B):
        m = dict(shared)
        m["xT"] = np.ascontiguousarray(x[b, :Tn].T)
        m["memT"] = np.ascontiguousarray(mem[b].T)
        maps.append(m)
    return maps


def run(inp, NT, NL, TT):
    inp = {k: np.asarray(v) for k, v in inp.items()}
    key = (NT, NL, TT)
    nc, es, n_ins = build(NT, NL, TT)
    maps = prep_inputs(inp, NT, NL, TT)
    res = run_bass_kernel_spmd(nc, maps, core_ids=list(range(len(maps))))
    out = np.stack([np.ascontiguousarray(r["outT"].T) for r in res.results])
    if DEBUG:
        global LAST_DBG
        LAST_DBG = np.stack([r["dbg"] for r in res.results])
    return out.astype(np.float32)


def kernel(**inputs):
    return run(inputs, SEQ // 256, DEPTH, 256)
```
